# Optimizing a Trainium2 kernel written in Bass

```python
import math
import jax
import jax.numpy as jnp
from jax import lax
import numpy as np

D_MODEL = 1024
BATCH = 16
SEQ = 4096
DEPTH = 2

MEM_LEN = 256
EPS = 1e-6
N_HEADS = 8
N_KV_HEADS = 2
HEAD_DIM = 64
ATTN_WIDTH = N_HEADS * HEAD_DIM
KV_WIDTH = N_KV_HEADS * HEAD_DIM
WINDOW = 128
BLOCK = 128
ROPE_THETA = 10000.0
HY_WIDTH = D_MODEL - ATTN_WIDTH
HY_ORDER = 2
HY_SHORT = 3
HY_EMB = 33
HY_BANDS = (HY_EMB - 1) // 2
HY_FILTER_HIDDEN = 64
HY_TARGET = 1e-2
HY_FAST_DECAY = 0.3
HY_SLOW_DECAY = 1.5
HY_MIN_DECAY = math.log(HY_TARGET) / HY_SLOW_DECAY
HY_MAX_DECAY = math.log(HY_TARGET) / HY_FAST_DECAY
MIX_WIDTH = ATTN_WIDTH + HY_WIDTH
Q_END = ATTN_WIDTH
K_END = Q_END + KV_WIDTH
V_END = K_END + KV_WIDTH
IN_COLS = V_END + (HY_ORDER + 1) * HY_WIDTH
X_HEADS = 4
X_HEAD_DIM = 128
X_WIDTH = X_HEADS * X_HEAD_DIM
D_FF = 2816
N_EXPERTS = 8
TOP_K = 2
D_FF_EXPERT = 3584
MOE_BLOCK = 512
N_DENSE = (DEPTH + 1) // 2
N_MOE = DEPTH // 2

kernel_name = 'hymba_attn_hyena_moe_encoder'


def rms_norm(x, g):
    xf = x.astype(jnp.float32)
    y = xf * lax.rsqrt(jnp.mean(xf * xf, axis=-1, keepdims=True) + EPS)
    return (y * g.astype(jnp.float32)).astype(x.dtype)


def rope_tables(seq):
    pos = jnp.arange(seq, dtype=jnp.float32)
    inv = ROPE_THETA ** (-jnp.arange(0, HEAD_DIM, 2, dtype=jnp.float32) / HEAD_DIM)
    ang = pos[:, None] * inv[None, :]
    return jnp.cos(ang), jnp.sin(ang)


def apply_rope(x, cos, sin):
    x1, x2 = jnp.split(x.astype(jnp.float32), 2, axis=-1)
    c = cos[None, :, None, :]
    s = sin[None, :, None, :]
    return jnp.concatenate([x1 * c - x2 * s, x2 * c + x1 * s], axis=-1).astype(x.dtype)


def band_attention(q, k, v, sink):
    B, S, H, Dh = q.shape
    nb = S // BLOCK
    G = H // N_KV_HEADS
    scale = Dh ** -0.5
    qb = q.reshape(B, nb, BLOCK, N_KV_HEADS, G, Dh).transpose(1, 0, 2, 3, 4, 5)
    kp = jnp.pad(k, ((0, 0), (BLOCK, BLOCK), (0, 0), (0, 0)))
    vp = jnp.pad(v, ((0, 0), (BLOCK, BLOCK), (0, 0), (0, 0)))
    sink_l = jnp.broadcast_to(sink.astype(jnp.float32).reshape(1, N_KV_HEADS, G, 1, 1),
                              (B, N_KV_HEADS, G, BLOCK, 1))
    neg = jnp.finfo(jnp.float32).min

    def one_block(args):
        i, qi = args
        kk = lax.dynamic_slice_in_dim(kp, i * BLOCK, 3 * BLOCK, axis=1)
        vv = lax.dynamic_slice_in_dim(vp, i * BLOCK, 3 * BLOCK, axis=1)
        s = jnp.einsum('bqkgd,bskd->bkgqs', qi, kk, preferred_element_type=jnp.float32) * scale
        qpos = i * BLOCK + jnp.arange(BLOCK)
        kpos = (i - 1) * BLOCK + jnp.arange(3 * BLOCK)
        mask = (jnp.abs(kpos[None, :] - qpos[:, None]) <= WINDOW) & (kpos[None, :] >= 0) & (kpos[None, :] < S)
        s = jnp.where(mask[None, None, None], s, neg)
        p = jax.nn.softmax(jnp.concatenate([s, sink_l], axis=-1), axis=-1)[..., :-1]
        return jnp.einsum('bkgqs,bskd->bqkgd', p.astype(vv.dtype), vv)

    o = lax.map(one_block, (jnp.arange(nb), qb))
    return o.transpose(1, 0, 2, 3, 4, 5).reshape(B, S, H * Dh)


def hyena_pos_features(L):
    t = jnp.linspace(0.0, 1.0, L, dtype=jnp.float32)[:, None]
    w = 2.0 * math.pi * jnp.arange(L, dtype=jnp.float32)[:, None] / L
    f = jnp.linspace(1e-4, HY_BANDS - 1, HY_BANDS, dtype=jnp.float32)[None, :]
    z = jnp.concatenate([t, jnp.cos(f * w), -jnp.sin(f * w)], axis=-1)
    return z, t


def hyena_filters(z, t, f_w1, f_b1, f_freq1, f_w2, f_b2, f_freq2, f_w3):
    L = z.shape[0]
    h = jnp.sin(f_freq1 * (z @ f_w1 + f_b1))
    h = jnp.sin(f_freq2 * (h @ f_w2 + f_b2))
    h = (h @ f_w3).astype(jnp.float32).reshape(L, 2, HY_ORDER, HY_WIDTH)
    deltas = jnp.linspace(HY_MIN_DECAY, HY_MAX_DECAY, HY_ORDER * HY_WIDTH,
                          dtype=jnp.float32).reshape(HY_ORDER, HY_WIDTH)
    h = h * jnp.exp(-t[:, :, None, None] * jnp.abs(deltas))
    fwd = h[:, 0]
    bwd = h[1:, 1]
    return jnp.concatenate([fwd, jnp.zeros((1,) + fwd.shape[1:], jnp.float32), bwd[::-1]], axis=0)


def hyena_mixer(u, conv_w, conv_b, k_long, skip):
    L = u.shape[1]
    up = jnp.pad(u, ((0, 0), (1, 1), (0, 0)))
    u = up[:, :-2] * conv_w[0] + up[:, 1:-1] * conv_w[1] + up[:, 2:] * conv_w[2] + conv_b
    x1, x2, v = jnp.split(u, HY_ORDER + 1, axis=-1)
    K = jnp.fft.rfft(k_long, axis=0)

    def long_conv(zin, Ko, d):
        zf32 = zin.astype(jnp.float32)
        zf = jnp.fft.rfft(zf32, n=2 * L, axis=1)
        y = jnp.fft.irfft(zf * Ko[None], n=2 * L, axis=1)[:, :L]
        return (y + zf32 * d.astype(jnp.float32)).astype(zin.dtype)

    zz = x1 * long_conv(v, K[:, 0], skip[0])
    return x2 * long_conv(zz, K[:, 1], skip[1])


def memory_attention(h, m, w_q, w_k, w_v, w_o):
    B, S, _ = h.shape
    q = (h @ w_q).reshape(B, S, X_HEADS, X_HEAD_DIM)
    k = (m @ w_k).reshape(B, -1, X_HEADS, X_HEAD_DIM)
    v = (m @ w_v).reshape(B, -1, X_HEADS, X_HEAD_DIM)
    s = jnp.einsum('bqhd,bkhd->bhqk', q, k, preferred_element_type=jnp.float32) * (X_HEAD_DIM ** -0.5)
    p = jax.nn.softmax(s, axis=-1).astype(v.dtype)
    o = jnp.einsum('bhqk,bkhd->bqhd', p, v).reshape(B, S, X_WIDTH)
    return o @ w_o


def swiglu(h, w_gate, w_up, w_down):
    return (jax.nn.silu(h @ w_gate) * (h @ w_up)) @ w_down


def moe_swiglu(h, w_router, w_gate, w_up, w_down):
    B, S, D = h.shape
    t = h.reshape(B * S, D)
    n_tok = B * S
    n_asg = n_tok * TOP_K
    logits = jnp.dot(t.astype(jnp.float32), w_router.astype(jnp.float32))
    top_v, top_i = lax.top_k(logits, TOP_K)
    top_w = jax.nn.softmax(top_v, axis=-1)
    flat_e = top_i.reshape(-1)
    flat_tok = jnp.repeat(jnp.arange(n_tok, dtype=jnp.int32), TOP_K)
    flat_w = top_w.reshape(-1)
    order = jnp.argsort(flat_e)
    sorted_e = flat_e[order]
    counts = jnp.bincount(flat_e, length=N_EXPERTS)
    padded = (counts + MOE_BLOCK - 1) // MOE_BLOCK * MOE_BLOCK
    pad_end = jnp.cumsum(padded)
    pad_start = pad_end - padded
    seg_start = jnp.cumsum(counts) - counts
    dest = pad_start[sorted_e] + jnp.arange(n_asg, dtype=jnp.int32) - seg_start[sorted_e]
    n_blk = -(-(n_asg + N_EXPERTS * (MOE_BLOCK - 1)) // MOE_BLOCK)
    buf_tok = jnp.zeros((n_blk * MOE_BLOCK,), jnp.int32).at[dest].set(flat_tok[order])
    buf_w = jnp.zeros((n_blk * MOE_BLOCK,), jnp.float32).at[dest].set(flat_w[order])
    blk_e = jnp.minimum(jnp.searchsorted(pad_end, jnp.arange(n_blk, dtype=jnp.int32) * MOE_BLOCK, side='right'),
                        N_EXPERTS - 1)

    def run_block(args):
        e, tok, wt = args
        xb = t[tok]
        yb = swiglu(xb, w_gate[e], w_up[e], w_down[e])
        return yb * wt[:, None].astype(yb.dtype)

    y = lax.map(run_block, (blk_e, buf_tok.reshape(n_blk, MOE_BLOCK), buf_w.reshape(n_blk, MOE_BLOCK)))
    out = jnp.zeros_like(t).at[buf_tok].add(y.reshape(-1, D))
    return out.reshape(B, S, D)


def setup_inputs(seed: int = 0) -> dict:
    key = jax.random.key(seed)
    ks = iter(jax.random.split(key, 40))

    def nrm(shape, scale):
        return scale * jax.random.normal(next(ks), shape, jnp.float32)

    def gain(shape):
        return 1.0 + 0.05 * jax.random.normal(next(ks), shape, jnp.float32)

    D = D_MODEL
    C3 = (HY_ORDER + 1) * HY_WIDTH
    return {
        'x': nrm((BATCH, SEQ, D), 1.0),
        'mem': nrm((BATCH, MEM_LEN, D), 1.0),
        'mem_norm': gain((D,)),
        'mix_norm': gain((DEPTH, D)),
        'w_in': nrm((DEPTH, D, IN_COLS), D ** -0.5),
        'attn_sink': nrm((DEPTH, N_HEADS), 0.5),
        'hy_conv_w': nrm((DEPTH, HY_SHORT, C3), HY_SHORT ** -0.5),
        'hy_conv_b': nrm((DEPTH, C3), 0.02),
        'hy_f_w1': nrm((DEPTH, HY_EMB, HY_FILTER_HIDDEN), HY_EMB ** -0.5),
        'hy_f_b1': nrm((DEPTH, HY_FILTER_HIDDEN), 0.1),
        'hy_f_freq1': gain((DEPTH, HY_FILTER_HIDDEN)),
        'hy_f_w2': nrm((DEPTH, HY_FILTER_HIDDEN, HY_FILTER_HIDDEN), HY_FILTER_HIDDEN ** -0.5),
        'hy_f_b2': nrm((DEPTH, HY_FILTER_HIDDEN), 0.1),
        'hy_f_freq2': gain((DEPTH, HY_FILTER_HIDDEN)),
        'hy_f_w3': nrm((DEPTH, HY_FILTER_HIDDEN, 2 * HY_ORDER * HY_WIDTH), 0.05 * HY_FILTER_HIDDEN ** -0.5),
        'hy_skip': nrm((DEPTH, HY_ORDER, HY_WIDTH), 0.5),
        'attn_out_norm': gain((DEPTH, ATTN_WIDTH)),
        'hy_out_norm': gain((DEPTH, HY_WIDTH)),
        'w_out': nrm((DEPTH, MIX_WIDTH, D), MIX_WIDTH ** -0.5),
        'xattn_norm': gain((DEPTH, D)),
        'xw_q': nrm((DEPTH, D, X_WIDTH), D ** -0.5),
        'xw_k': nrm((DEPTH, D, X_WIDTH), D ** -0.5),
        'xw_v': nrm((DEPTH, D, X_WIDTH), D ** -0.5),
        'xw_o': nrm((DEPTH, X_WIDTH, D), X_WIDTH ** -0.5),
        'ffn_norm': gain((DEPTH, D)),
        'ffn_w_gate': nrm((N_DENSE, D, D_FF), D ** -0.5),
        'ffn_w_up': nrm((N_DENSE, D, D_FF), D ** -0.5),
        'ffn_w_down': nrm((N_DENSE, D_FF, D), D_FF ** -0.5),
        'moe_router': nrm((N_MOE, D, N_EXPERTS), D ** -0.5),
        'moe_w_gate': nrm((N_MOE, N_EXPERTS, D, D_FF_EXPERT), D ** -0.5),
        'moe_w_up': nrm((N_MOE, N_EXPERTS, D, D_FF_EXPERT), D ** -0.5),
        'moe_w_down': nrm((N_MOE, N_EXPERTS, D_FF_EXPERT, D), D_FF_EXPERT ** -0.5),
        'final_norm': gain((D,)),
    }


def reference(x, mem, mem_norm, mix_norm, w_in, attn_sink, hy_conv_w, hy_conv_b,
              hy_f_w1, hy_f_b1, hy_f_freq1, hy_f_w2, hy_f_b2, hy_f_freq2, hy_f_w3, hy_skip,
              attn_out_norm, hy_out_norm, w_out, xattn_norm, xw_q, xw_k, xw_v, xw_o,
              ffn_norm, ffn_w_gate, ffn_w_up, ffn_w_down,
              moe_router, moe_w_gate, moe_w_up, moe_w_down, final_norm):
    B, S, _ = x.shape
    cos, sin = rope_tables(S)
    z_pos, t_pos = hyena_pos_features(S)
    m = rms_norm(mem, mem_norm)
    for l in range(DEPTH):
        h = rms_norm(x, mix_norm[l])
        proj = h @ w_in[l]
        q, k, v, u = jnp.split(proj, [Q_END, K_END, V_END], axis=-1)
        q = apply_rope(q.reshape(B, S, N_HEADS, HEAD_DIM), cos, sin)
        k = apply_rope(k.reshape(B, S, N_KV_HEADS, HEAD_DIM), cos, sin)
        v = v.reshape(B, S, N_KV_HEADS, HEAD_DIM)
        a = band_attention(q, k, v, attn_sink[l])
        k_long = hyena_filters(z_pos, t_pos, hy_f_w1[l], hy_f_b1[l], hy_f_freq1[l],
                               hy_f_w2[l], hy_f_b2[l], hy_f_freq2[l], hy_f_w3[l])
        y = hyena_mixer(u, hy_conv_w[l], hy_conv_b[l], k_long, hy_skip[l])
        mixed = jnp.concatenate([rms_norm(a, attn_out_norm[l]), rms_norm(y, hy_out_norm[l])], axis=-1)
        x = x + mixed @ w_out[l]
        x = x + memory_attention(rms_norm(x, xattn_norm[l]), m, xw_q[l], xw_k[l], xw_v[l], xw_o[l])
        h = rms_norm(x, ffn_norm[l])
        if l % 2 == 0:
            j = l // 2
            x = x + swiglu(h, ffn_w_gate[j], ffn_w_up[j], ffn_w_down[j])
        else:
            j = l // 2
            x = x + moe_swiglu(h, moe_router[j], moe_w_gate[j], moe_w_up[j], moe_w_down[j])
    return rms_norm(x, final_norm)
```

```python
import math
from contextlib import ExitStack
import numpy as np
import ml_dtypes
import concourse.bass as bass
import concourse.mybir as mybir
from concourse.bass_utils import run_bass_kernel_spmd

F32 = mybir.dt.float32
BF16 = mybir.dt.bfloat16
AF = mybir.ActivationFunctionType
ALU = mybir.AluOpType
BF = ml_dtypes.bfloat16

D = 1024
L = 4096
NBC = 2
NCORES = 8
MEM = 256
DFF = 2816
NE = 8
DFE = 3584
EPS = 1e-6
NBLK = NBC * L * 2 // 512 + NE
NWA = 23
PI = math.pi


class Sched:
    NDS = 8

    def __init__(self, nc):
        self.nc = nc
        self.eng = {'pe': nc.tensor, 'act': nc.scalar, 'dve': nc.vector, 'pool': nc.gpsimd, 'sp': nc.sync}
        self.sem = {e: nc.alloc_semaphore(f"s_{e}") for e in ('pe', 'act', 'dve', 'pool')}
        self.cnt = {e: 0 for e in self.sem}
        self.dsem = {q: [nc.alloc_semaphore(f"d_{q}{i}") for i in range(self.NDS)] for q in ('sp', 'pool', 'act')}
        self.dcnt = {(q, i): 0 for q in self.dsem for i in range(self.NDS)}
        self.dnext = {q: 0 for q in self.dsem}
        self.waited = {}
        self.last_w = {}
        self.reads = {}
        self.pe_pending = ([], [])

    def _wait(self, e, tok):
        if tok[0] == 'c':
            _, e2, c = tok
            if e2 == e and e == 'pe':
                return
            key = (e, 'c', e2)
            if self.waited.get(key, 0) >= c:
                return
            self.waited[key] = c
            self.eng[e].wait_ge(self.sem[e2], c)
        else:
            _, q, i, c = tok
            key = (e, 'd', q, i)
            if self.waited.get(key, 0) >= c:
                return
            self.waited[key] = c
            self.eng[e].wait_ge(self.dsem[q][i], 16 * c)

    def _deps(self, e, reads, writes):
        toks = []
        for r in reads:
            if r in self.last_w:
                toks.append(self.last_w[r])
        for w in writes:
            if w in self.last_w:
                toks.append(self.last_w[w])
            toks.extend(self.reads.get(w, []))
        for t in toks:
            self._wait(e, t)

    def _record(self, tok, reads, writes):
        for w in writes:
            self.last_w[w] = tok
            self.reads[w] = []
        for r in reads:
            if r not in writes:
                self.reads.setdefault(r, []).append(tok)

    def op(self, e, fn, reads=(), writes=(), signal=True):
        reads = list(reads)
        writes = list(writes)
        self._deps(e, reads, writes)
        ins = fn()
        if e == 'pe' and not signal:
            self.pe_pending[0].extend(reads)
            self.pe_pending[1].extend(writes)
            return ins
        ins.then_inc(self.sem[e], 1)
        self.cnt[e] += 1
        tok = ('c', e, self.cnt[e])
        if e == 'pe':
            reads += self.pe_pending[0]
            writes += self.pe_pending[1]
            self.pe_pending = ([], [])
        self._record(tok, reads, writes)
        return ins

    def dma(self, q, out, in_, reads=(), writes=(), **kw):
        i = self.dnext[q]
        self.dnext[q] = (i + 1) % self.NDS
        c = self.dcnt[(q, i)]
        if c > 0:
            self._wait(q, ('d', q, i, c))
        self._deps(q, reads, writes)
        ins = self.eng[q].dma_start(out=out, in_=in_, **kw)
        ins.then_inc(self.dsem[q][i], 16)
        self.dcnt[(q, i)] = c + 1
        tok = ('d', q, i, c + 1)
        self._record(tok, list(reads), list(writes))
        return ins

    def idma(self, out, out_offset, in_, in_offset, reads=(), writes=()):
        q = 'pool'
        i = self.dnext[q]
        self.dnext[q] = (i + 1) % self.NDS
        c = self.dcnt[(q, i)]
        if c > 0:
            self._wait(q, ('d', q, i, c))
        self._deps(q, reads, writes)
        ins = self.nc.gpsimd.indirect_dma_start(out=out, out_offset=out_offset, in_=in_, in_offset=in_offset)
        ins.then_inc(self.dsem[q][i], 16)
        self.dcnt[(q, i)] = c + 1
        tok = ('d', q, i, c + 1)
        self._record(tok, list(reads), list(writes))
        return ins

    def barrier(self):
        assert not self.pe_pending[0] and not self.pe_pending[1]
        for e in self.eng:
            for e2 in self.sem:
                if e2 != e and self.cnt[e2] > 0:
                    self._wait(e, ('c', e2, self.cnt[e2]))
            for (q, i), c in self.dcnt.items():
                if c > 0:
                    self._wait(e, ('d', q, i, c))
        self.last_w = {}
        self.reads = {}


_CONST = {}


def host_constants():
    if _CONST:
        return _CONST
    c = {}
    c['identb'] = np.eye(128, dtype=np.float32).astype(BF)
    kt = np.arange(128)[:, None]
    qt = np.arange(128)[None, :]
    c['mprev'] = (kt >= qt).astype(np.float32).astype(BF)
    c['mnext'] = (kt <= qt).astype(np.float32).astype(BF)
    pos = np.arange(L, dtype=np.float32)
    inv = (10000.0 ** (-np.arange(0, 64, 2, dtype=np.float32) / 64)).astype(np.float32)
    ang = pos[:, None] * inv[None, :]
    cos, sin = np.cos(ang).astype(np.float32), np.sin(ang).astype(np.float32)
    dh = np.arange(128) % 64
    c['ropec'] = np.ascontiguousarray(cos[:, dh % 32].T).astype(BF)
    sgn = np.where(dh < 32, -1.0, 1.0).astype(np.float32)
    c['ropes'] = np.ascontiguousarray((sin[:, dh % 32] * sgn[None, :]).T).astype(BF)
    N = 2 * L
    fperm = np.concatenate([np.arange(0, L, 2), np.arange(1, L, 2)]).astype(np.int64)
    idx = (np.arange(L, dtype=np.int64)[:, None] * fperm[None, :]) % N
    ang = idx.astype(np.float64) * (2 * np.pi / N)
    c['dftc'] = np.cos(ang).astype(np.float32).astype(BF)
    c['dfts'] = np.sin(ang).astype(np.float32).astype(BF)
    alt = np.where(np.arange(L) % 2 == 0, 1.0, -1.0)
    idx2 = ((2 * np.arange(L // 2, dtype=np.int64)[:, None] + 1) * fperm[None, :]) % (2 * N)
    ang2 = idx2.astype(np.float64) * (2 * np.pi / (2 * N))
    cf = np.cos(ang2)
    sf = np.sin(ang2)
    sf[:, 0] = alt[:L // 2]
    c['dfcf'] = cf.astype(np.float32).astype(BF)
    c['dfsf'] = sf.astype(np.float32).astype(BF)
    c['dfci'] = np.ascontiguousarray(cf.T).astype(np.float32).astype(BF)
    c['dfsi'] = np.ascontiguousarray(sf.T).astype(np.float32).astype(BF)
    nq = np.zeros((L, 128), np.float32)
    nq[:, 0] = alt
    c['dftnq'] = nq.astype(BF)
    w = np.full((L,), 2.0 / N, np.float32)
    w[0] = 1.0 / N
    c['wcol'] = np.ascontiguousarray(w.reshape(32, 128).T)
    t = np.linspace(0.0, 1.0, L, dtype=np.float32)[:, None]
    wv = (2.0 * np.float32(math.pi) * np.arange(L, dtype=np.float32)[:, None] / np.float32(L)).astype(np.float32)
    f = np.linspace(1e-4, 15.0, 16, dtype=np.float32)[None, :]
    z = np.concatenate([t, np.cos(f * wv), -np.sin(f * wv)], axis=-1).astype(np.float32)
    c['zT'] = np.ascontiguousarray(z.T)
    mn = math.log(1e-2) / 1.5
    mx = math.log(1e-2) / 0.3
    deltas = np.linspace(mn, mx, 1024, dtype=np.float32)
    dec = np.exp(-t * np.abs(deltas)[None, :]).astype(np.float32)
    dec4 = np.concatenate([dec, dec], axis=1)
    dec4[0, 1024:] = 0.0
    c['decay'] = np.ascontiguousarray(dec4)
    drev = np.zeros((L // 2, 2048), np.float32)
    drev[1:] = dec4[L - 1:L // 2:-1]
    c['decay_rev'] = drev
    k_ = np.arange(128)[:, None]
    m_ = np.arange(128)[None, :]
    c['ltb'] = (k_ < m_).astype(np.float32).astype(BF)
    slt = (np.arange(64)[:, None] < np.arange(65)[None, :]).astype(np.float32)
    slt[:, 64] = 1.0
    c['slt65'] = slt.astype(BF)
    c['rowbase'] = (128.0 * np.arange(DFE // 128, dtype=np.float32)[None, :] + np.arange(128, dtype=np.float32)[:, None]).astype(np.float32)
    c['thr32'] = np.tile((512.0 * np.arange(32, dtype=np.float32))[None, :], (128, 1))
    c['thr40'] = np.tile((512.0 * np.arange(NBLK, dtype=np.float32) + 0.5)[None, :], (128, 1))
    _CONST.update(c)
    return c


def permute_w_in(w_in_l):
    cols = []
    for swap in (0, 1):
        for g in range(4):
            for kvh in range(2):
                h = kvh * 4 + g
                dh = (np.arange(64) + 32 * swap) % 64
                cols.append(h * 64 + dh)
    q = np.concatenate(cols)
    kc = []
    for swap in (0, 1):
        for kvh in range(2):
            dh = (np.arange(64) + 32 * swap) % 64
            kc.append(512 + kvh * 64 + dh)
    order = np.concatenate([q, np.concatenate(kc), np.arange(640, 768), np.arange(768, 2304)])
    return np.ascontiguousarray(w_in_l[:, order])


def attn_row_perm():
    rows = []
    for g in range(4):
        for kvh in range(2):
            rows.append((kvh * 4 + g) * 64 + np.arange(64))
    return np.concatenate(rows)


def build_program(n_layers=2, taps=(), debug=False):
    nc = bass.Bass("TRN2", target_bir_lowering=False)
    S = Sched(nc)
    I = {}

    def din(name, shape, dt=F32):
        I[name] = nc.dram_tensor(name, list(shape), dt, kind="ExternalInput").ap()
        return I[name]

    def dscr(name, shape, dt):
        return nc.dram_tensor(name, list(shape), dt, kind=("ExternalOutput" if debug else "Internal")).ap()

    x_in = din('x', [NBC, L, D])
    mem_in = din('mem', [NBC * MEM, D])
    din('mem_norm', [D]); din('mix_norm', [2, D]); din('wa', [2, D, NWA * 128]); din('attn_sink', [2, 8])
    din('cw', [2, 3, 1536]); din('cb', [2, 1536])
    din('f_w1', [2, 33, 64]); din('f_b1', [2, 64]); din('f_fr1', [2, 64]); din('f_w2', [2, 64, 64])
    din('f_b2', [2, 64]); din('f_fr2', [2, 64]); din('f_w3', [2, 64, 2048]); din('skip', [2, 2, 512])
    din('ga', [2, 512]); din('gy', [2, 512]); din('w_out', [2, D, D]); din('xattn_norm', [2, D])
    din('xw_q', [2, D, 512]); din('xw_k', [2, D, 512]); din('xw_v', [2, D, 512]); din('xw_o', [2, 512, D])
    din('ffn_norm', [2, D]); din('ffn_wg', [DFF, D]); din('ffn_wu', [DFF, D]); din('ffn_wd', [DFF, D])
    din('moe_router', [D, NE]); din('moe_wg', [NE * DFE, D]); din('moe_wu', [NE * DFE, D]); din('moe_wd', [NE * DFE, D])
    din('final_norm', [D])
    din('identb', [128, 128], BF16); din('mprev', [128, 128], BF16); din('mnext', [128, 128], BF16)
    din('ropec', [128, L], BF16); din('ropes', [128, L], BF16)
    din('dftc', [L, L], BF16); din('dfts', [L, L], BF16); din('dftnq', [L, 128], BF16)
    din('dfcf', [L // 2, L], BF16); din('dfsf', [L // 2, L], BF16); din('dfci', [L, L // 2], BF16); din('dfsi', [L, L // 2], BF16)
    din('wcol', [128, 32]); din('zT', [33, L]); din('decay', [L, 2048]); din('decay_rev', [L // 2, 2048])
    din('ltb', [128, 128], BF16); din('slt65', [64, 65], BF16); din('thr32', [128, 32]); din('thr40', [128, NBLK]); din('rowbase', [128, DFE // 128])
    y_out = nc.dram_tensor('y', [NBC, L, D], F32, kind="ExternalOutput").ap()

    xs = dscr('xs', [NBC, L, D], F32)
    U_d = dscr('U_d', [NBC, 12, 128, L + 8], BF16)
    ZZ_d = dscr('ZZ_d', [4, 128, L], BF16)
    aT_d = dscr('aT_d', [NBC, 128, 4, L], BF16)
    yT_d = dscr('yT_d', [NBC, 128, 4, L], BF16)
    AB_d = dscr('AB_d', [2, 2, L, 512], BF16)
    H2_d = dscr('H2_d', [NBC * L, D], BF16)
    G_d = dscr('G_d', [NBLK * 512, D], BF16)
    Y_d = dscr('Y_d', [NBLK * 512, D], F32)
    tapd = {}
    for nm, shape, dt in taps:
        tapd[nm] = nc.dram_tensor('tap_' + nm, list(shape), dt, kind="ExternalOutput").ap()

    V, A, P, T = nc.vector, nc.scalar, nc.gpsimd, nc.tensor
    uctr = [0]

    def UN(n):
        uctr[0] += 1
        return f"{n}_{uctr[0]}"

    with ExitStack() as es_:
        identb = es_.enter_context(nc.sbuf_tensor(UN("identb"), [128, 128], BF16))
        mprev = es_.enter_context(nc.sbuf_tensor(UN("mprev"), [128, 128], BF16))
        mnext = es_.enter_context(nc.sbuf_tensor(UN("mnext"), [128, 128], BF16))
        onesb = es_.enter_context(nc.sbuf_tensor(UN("onesb"), [128, 128], BF16))
        onesm = es_.enter_context(nc.sbuf_tensor(UN("onesm"), [128, 2, 128], BF16))
        epsc = es_.enter_context(nc.sbuf_tensor(UN("epsc"), [128, 1], F32))
        npi = es_.enter_context(nc.sbuf_tensor(UN("npi"), [128, 1], F32))
        mT = es_.enter_context(nc.sbuf_tensor(UN("mT"), [128, 8, NBC * MEM], BF16))
        KmT = es_.enter_context(nc.sbuf_tensor(UN("KmT"), [128, 4, NBC * MEM], BF16))
        Vm = es_.enter_context(nc.sbuf_tensor(UN("Vm"), [128, NBC * 2, 512], BF16))
        ss = es_.enter_context(nc.sbuf_tensor(UN("ss"), [128, 8], F32))
        gbc = es_.enter_context(nc.sbuf_tensor(UN("gbc"), [128, D], F32))
        ps0 = es_.enter_context(nc.psum_tensor(UN("ps0"), [128, 512], F32))
        ps1 = es_.enter_context(nc.psum_tensor(UN("ps1"), [128, 512], F32))
        ps2 = es_.enter_context(nc.psum_tensor(UN("ps2"), [128, 512], F32))
        ps3 = es_.enter_context(nc.psum_tensor(UN("ps3"), [128, 512], F32))
        ps4 = es_.enter_context(nc.psum_tensor(UN("ps4"), [128, 512], F32))
        ps5 = es_.enter_context(nc.psum_tensor(UN("ps5"), [128, 512], F32))
        ps6 = es_.enter_context(nc.psum_tensor(UN("ps6"), [128, 512], F32))
        ps7 = es_.enter_context(nc.psum_tensor(UN("ps7"), [128, 512], F32))
        PS = [ps0, ps1, ps2, ps3, ps4, ps5, ps6, ps7]

        def psk(i):
            return ('ps', i)

        def load_gain(vec_ap):
            with nc.allow_non_contiguous_dma(reason="bcast"):
                S.dma('sp', gbc[:, :], vec_ap.partition_broadcast(128), writes=['gbc'])

        ss_ctr = [0]

        def norm_T(x_ap, xn_ap, xnk, hT_ap, hTk, bank, xk, gt=None, gk='gbc'):
            si = ss_ctr[0] % 8
            ss_ctr[0] += 1
            sc = ss[:, si:si + 1]
            sk = ('ss', si)
            gt = gbc if gt is None else gt
            S.op('act', lambda: A.activation(out=junk[:, :], in_=x_ap, func=AF.Square, accum_out=sc),
                 reads=[xk], writes=['junk', sk])
            S.op('act', lambda: A.activation(out=sc, in_=sc, func=AF.Ln, bias=epsc[:, 0:1], scale=1.0 / D), reads=[sk], writes=[sk])
            S.op('act', lambda: A.activation(out=sc, in_=sc, func=AF.Exp, scale=-0.5), reads=[sk], writes=[sk])
            S.op('dve', lambda: V.scalar_tensor_tensor(out=xn_ap, in0=x_ap, scalar=sc, in1=gt[:, :], op0=ALU.mult, op1=ALU.mult),
                 reads=[xk, sk, gk], writes=[xnk])
            pb = PS[bank][:, :].bitcast(BF16)
            for k in range(8):
                S.op('pe', lambda: T.transpose(pb[:, k * 128:(k + 1) * 128], xn_ap[:, k * 128:(k + 1) * 128], identb[:, :]),
                     reads=[xnk, 'identb'], writes=[psk(bank)], signal=(k == 7))
            S.op('act', lambda: A.copy(out=hT_ap, in_=pb.rearrange("p (k t) -> p k t", k=8)), reads=[psk(bank)], writes=[hTk])

        def norm_T_batch(items, gt=None, gk='gbc'):
            gt = gbc if gt is None else gt
            scs = []
            for (x_ap, xk, xn_ap, xnk, hT_ap, hTk, bank) in items:
                si = ss_ctr[0] % 8
                ss_ctr[0] += 1
                sc, sk = ss[:, si:si + 1], ('ss', si)
                scs.append((sc, sk))
                S.op('act', lambda: A.activation(out=junk[:, :], in_=x_ap, func=AF.Square, accum_out=sc), reads=[xk], writes=[sk])
                S.op('act', lambda: A.activation(out=sc, in_=sc, func=AF.Ln, bias=epsc[:, 0:1], scale=1.0 / D), reads=[sk], writes=[sk])
                S.op('act', lambda: A.activation(out=sc, in_=sc, func=AF.Exp, scale=-0.5), reads=[sk], writes=[sk])
            for (x_ap, xk, xn_ap, xnk, hT_ap, hTk, bank), (sc, sk) in zip(items, scs):
                S.op('dve', lambda: V.scalar_tensor_tensor(out=xn_ap, in0=x_ap, scalar=sc, in1=gt[:, :], op0=ALU.mult, op1=ALU.mult),
                     reads=[xk, sk, gk], writes=[xnk])
            for (x_ap, xk, xn_ap, xnk, hT_ap, hTk, bank) in items:
                pb = PS[bank][:, :].bitcast(BF16)
                for k in range(8):
                    S.op('pe', lambda: T.transpose(pb[:, k * 128:(k + 1) * 128], xn_ap[:, k * 128:(k + 1) * 128], identb[:, :]),
                         reads=[xnk, 'identb'], writes=[psk(bank)], signal=(k == 7))
                S.op('act', lambda: A.copy(out=hT_ap, in_=pb.rearrange("p (k t) -> p k t", k=8)), reads=[psk(bank)], writes=[hTk])

        def mm_group(bank, pairs, rk, col0=0, ncol=512):
            n = len(pairs)
            for i, (lt, rh) in enumerate(pairs):
                S.op('pe', lambda: T.matmul(PS[bank][:, col0:col0 + ncol], lt, rh, start=(i == 0), stop=(i == n - 1)),
                     reads=rk, writes=[psk(bank)], signal=(i == n - 1))

        def phase_mem():
            with ExitStack() as es_:
                mx = es_.enter_context(nc.sbuf_tensor(UN("mx"), [128, 4, D], F32))
                mxn = es_.enter_context(nc.sbuf_tensor(UN("mxn"), [128, D], BF16))
                load_gain(I['mem_norm'])
                S.dma('sp', mx[:, :, :], mem_in.rearrange("(n p) d -> p n d", p=128), writes=['mx'])
                for n in range(4):
                    norm_T(mx[:, n, :], mxn[:, :], 'mxn', mT[:, :, n * 128:(n + 1) * 128], ('mT', n), n % 2, 'mx')
                S.barrier()

        def phase_F(l):
            with ExitStack() as es_:
                zTs = es_.enter_context(nc.sbuf_tensor(UN("zTs"), [33, 2, 512], F32))
                fw1 = es_.enter_context(nc.sbuf_tensor(UN("fw1"), [33, 64], F32))
                fw2 = es_.enter_context(nc.sbuf_tensor(UN("fw2"), [64, 64], F32))
                fw3 = es_.enter_context(nc.sbuf_tensor(UN("fw3"), [64, 2048], F32))
                fcol = es_.enter_context(nc.sbuf_tensor(UN("fcol"), [64, 8], F32))
                h1T = es_.enter_context(nc.sbuf_tensor(UN("h1T"), [64, L], F32))
                h2T = es_.enter_context(nc.sbuf_tensor(UN("h2T"), [64, L], F32))
                ftmp = es_.enter_context(nc.sbuf_tensor(UN("ftmp"), [64, 3, 512], F32))
                eb = es_.enter_context(nc.sbuf_tensor(UN("eb"), [128, 32, 512], BF16))
                ob = es_.enter_context(nc.sbuf_tensor(UN("ob"), [128, 32, 512], BF16))
                dct = es_.enter_context(nc.sbuf_tensor(UN("dct"), [128, 2, 2, 512], F32))
                kft = es_.enter_context(nc.sbuf_tensor(UN("kft"), [128, 2, 2, 512], F32))
                skr = es_.enter_context(nc.sbuf_tensor(UN("skr"), [1, 2, 512], F32))
                slab = es_.enter_context(nc.sbuf_tensor(UN("slab"), [128, 4, 2, 512], BF16))
                nqs = es_.enter_context(nc.sbuf_tensor(UN("nqs"), [128, 16, 128], BF16))
                d2048 = es_.enter_context(nc.sbuf_tensor(UN("d2048"), [1, 2, 512], F32))
                k2048 = es_.enter_context(nc.sbuf_tensor(UN("k2048"), [1, 2, 512], F32))
                eo2048 = es_.enter_context(nc.sbuf_tensor(UN("eo2048"), [1, 2, 512], BF16))
                rows = es_.enter_context(nc.sbuf_tensor(UN("rows"), [1, 2, 2, 512], BF16))
                wcol = es_.enter_context(nc.sbuf_tensor(UN("wcol"), [128, 32], F32))
                abo = es_.enter_context(nc.sbuf_tensor(UN("abo"), [128, 2, 2, 512], BF16))
                S.dma('sp', fw1[:, :], I['f_w1'][l], writes=['fw'])
                S.dma('sp', fw2[:, :], I['f_w2'][l], writes=['fw'])
                S.dma('sp', fw3[:, :], I['f_w3'][l], writes=['fw'])
                S.dma('sp', wcol[:, :], I['wcol'], writes=['wcol'])
                S.dma('sp', nqs[:, :, :], I['dftnq'][0:L // 2, :].rearrange("(k p) f -> p k f", p=128), writes=['nqs'])
                S.dma('sp', skr[:, :, :], I['skip'][l].unsqueeze(0), writes=['skr'])
                with nc.allow_non_contiguous_dma(reason="tiny"):
                    for ci, nm in enumerate(('f_b1', 'f_fr1', 'f_b2', 'f_fr2')):
                        S.dma('sp', fcol[:, ci:ci + 1], I[nm][l].unsqueeze(1), writes=['fcol'])
                S.op('dve', lambda: V.tensor_tensor(out=fcol[:, 4:5], in0=fcol[:, 0:1], in1=fcol[:, 1:2], op=ALU.mult), reads=['fcol'], writes=['fcol'])
                S.op('dve', lambda: V.tensor_tensor(out=fcol[:, 5:6], in0=fcol[:, 2:3], in1=fcol[:, 3:4], op=ALU.mult), reads=['fcol'], writes=['fcol'])

                def sin_layer(w_ap, src, dst, frc, fbc, kdim):
                    for n in range(8):
                        bank = n % 2
                        if src is None:
                            S.dma('sp', zTs[:, n % 2, :], I['zT'][:, n * 512:(n + 1) * 512], writes=[('zTs', n % 2)])
                            rhs_ap = zTs[:, n % 2, :]
                        else:
                            rhs_ap = src[0:kdim, n * 512:(n + 1) * 512]
                        S.op('pe', lambda: T.matmul(PS[bank][0:64, :], w_ap, rhs_ap, start=True, stop=True),
                             reads=['fw', ('zTs', n % 2), 'h1T'], writes=[psk(bank)])
                        t0, t1, t2 = ftmp[:, 0, :], ftmp[:, 1, :], ftmp[:, 2, :]
                        S.op('dve', lambda: V.tensor_scalar(out=t0, in0=PS[bank][0:64, :], scalar1=fcol[:, frc:frc + 1], scalar2=fcol[:, fbc:fbc + 1], op0=ALU.mult, op1=ALU.add),
                             reads=[psk(bank), 'fcol'], writes=['ft0'])
                        S.op('dve', lambda: V.tensor_scalar(out=t1, in0=t0, scalar1=PI, scalar2=-2 * PI, op0=ALU.is_gt, op1=ALU.mult), reads=['ft0'], writes=['ft1'])
                        S.op('dve', lambda: V.tensor_scalar(out=t2, in0=t0, scalar1=-PI, scalar2=2 * PI, op0=ALU.is_lt, op1=ALU.mult), reads=['ft0'], writes=['ft2'])
                        S.op('dve', lambda: V.tensor_tensor(out=t1, in0=t1, in1=t2, op=ALU.add), reads=['ft1', 'ft2'], writes=['ft1'])
                        S.op('dve', lambda: V.tensor_tensor(out=t0, in0=t0, in1=t1, op=ALU.add), reads=['ft0', 'ft1'], writes=['ft0'])
                        S.op('act', lambda: A.activation(out=dst[:, n * 512:(n + 1) * 512], in_=t0, func=AF.Sin), reads=['ft0'], writes=['h1T' if dst is h1T else 'h2T'])

                sin_layer(fw1[:, :], None, h1T, 1, 4, 33)
                sin_layer(fw2[:, :], h1T, h2T, 3, 5, 64)

                h2Tr = h1T
                S.op('dve', lambda: V.memset(h2Tr[:, 0:1], 0.0), reads=['h1T'], writes=['h1T'])
                S.op('dve', lambda: V.tensor_copy(out=h2Tr[:, 1:2048], in_=h2T[:, 2049:4096][:, ::-1]), reads=['h2T', 'h1T'], writes=['h1T'])
                for o in range(2):
                    for tc in range(16):
                        S.dma('sp', dct[:, 0, :, :], I['decay'][tc * 128:(tc + 1) * 128, :].rearrange("p (d o c) -> p d o c", d=2, o=2)[:, :, o, :], writes=[('dct', 0)])
                        S.dma('sp', dct[:, 1, :, :], I['decay_rev'][tc * 128:(tc + 1) * 128, :].rearrange("p (d o c) -> p d o c", d=2, o=2)[:, :, o, :], writes=[('dct', 1)])
                        for r, hsrc in enumerate((h2T, h2Tr)):
                            for d in range(2):
                                bank = r * 2 + d
                                S.op('pe', lambda: T.matmul(PS[bank][:, :], hsrc[:, tc * 128:(tc + 1) * 128], fw3[:, d * 1024 + o * 512: d * 1024 + o * 512 + 512], start=True, stop=True),
                                     reads=['h2T', 'h1T', 'fw'], writes=[psk(bank)])
                                S.op('dve', lambda: V.tensor_tensor(out=kft[:, r, d, :], in0=PS[bank][:, :], in1=dct[:, r, d, :], op=ALU.mult),
                                     reads=[psk(bank), ('dct', r)], writes=[('kft', r, d)])
                        if tc == 0:
                            S.op('dve', lambda: V.tensor_tensor(out=kft[0:1, 0, 0, :], in0=kft[0:1, 0, 0, :], in1=skr[0:1, o, :], op=ALU.add),
                                 reads=[('kft', 0, 0), 'skr'], writes=[('kft', 0, 0)])
                        kk = [('kft', r, d) for r in range(2) for d in range(2)]
                        for r in range(2):
                            S.op('dve', lambda: V.tensor_tensor(out=dct[:, r, 0, :], in0=kft[:, r, 0, :], in1=kft[:, r, 1, :], op=ALU.add), reads=kk + [('dct', r)], writes=[('dct', r)])
                            S.op('dve', lambda: V.tensor_tensor(out=dct[:, r, 1, :], in0=kft[:, r, 0, :], in1=kft[:, r, 1, :], op=ALU.subtract), reads=kk + [('dct', r)], writes=[('dct', r)])
                        dk = [('dct', 0), ('dct', 1)]
                        S.op('dve', lambda: V.tensor_tensor(out=eb[:, tc, :], in0=dct[:, 0, 0, :], in1=dct[:, 1, 0, :], op=ALU.add), reads=dk, writes=['eb'])
                        S.op('dve', lambda: V.tensor_tensor(out=eb[:, 16 + tc, :], in0=dct[:, 0, 0, :], in1=dct[:, 1, 0, :], op=ALU.subtract), reads=dk, writes=['eb'])
                        S.op('dve', lambda: V.tensor_tensor(out=ob[:, tc, :], in0=dct[:, 0, 1, :], in1=dct[:, 1, 1, :], op=ALU.add), reads=dk, writes=['ob'])
                        S.op('dve', lambda: V.tensor_tensor(out=ob[:, 16 + tc, :], in0=dct[:, 0, 1, :], in1=dct[:, 1, 1, :], op=ALU.subtract), reads=dk, writes=['ob'])
                    S.dma('sp', d2048[:, :, :], I['decay'][2048:2049, :].rearrange("p (d o c) -> p d o c", d=2, o=2)[:, :, o, :], writes=['d2048'])
                    for d in range(2):
                        S.op('pe', lambda: T.matmul(PS[d][0:1, :], h2T[:, 2048:2049], fw3[:, d * 1024 + o * 512: d * 1024 + o * 512 + 512], start=True, stop=True),
                             reads=['h2T', 'fw'], writes=[psk(d)])
                        S.op('dve', lambda: V.tensor_tensor(out=k2048[:, d, :], in0=PS[d][0:1, :], in1=d2048[:, d, :], op=ALU.mult), reads=[psk(d), 'd2048'], writes=['k2048'])
                    S.op('dve', lambda: V.tensor_tensor(out=eo2048[:, 0, :], in0=k2048[:, 0, :], in1=k2048[:, 1, :], op=ALU.add), reads=['k2048'], writes=['eo2048'])
                    S.op('dve', lambda: V.tensor_tensor(out=eo2048[:, 1, :], in0=k2048[:, 0, :], in1=k2048[:, 1, :], op=ALU.subtract), reads=['k2048'], writes=['eo2048'])
                    for fp in range(8):
                        even = fp < 4
                        S.dma('sp', rows[:, fp % 2, 0, :], I['dftc'][2048:2049, fp * 512:(fp + 1) * 512], writes=[('rows', fp % 2)])
                        S.dma('sp', rows[:, fp % 2, 1, :], I['dfts'][2048:2049, fp * 512:(fp + 1) * 512], writes=[('rows', fp % 2)])
                        for tc in range(16):
                            sl = (fp * 16 + tc) % 4
                            S.dma('sp', slab[:, sl, 0, :], I['dftc'][tc * 128:(tc + 1) * 128, fp * 512:(fp + 1) * 512], writes=[('slab', sl, 0)])
                            S.dma('sp', slab[:, sl, 1, :], I['dfts'][tc * 128:(tc + 1) * 128, fp * 512:(fp + 1) * 512], writes=[('slab', sl, 1)])
                            rc = eb[:, tc if even else 16 + tc, :]
                            rs = ob[:, 16 + tc if even else tc, :]
                            for fc in range(4):
                                S.op('pe', lambda: T.matmul(PS[fc][:, :], slab[:, sl, 0, fc * 128:(fc + 1) * 128], rc, start=(tc == 0), stop=(tc == 15 and not even)),
                                     reads=[('slab', sl, 0), 'eb'], writes=[psk(fc)], signal=(tc == 15))
                                last_s = (tc == 15) and even and not (fp == 0 and fc == 0)
                                S.op('pe', lambda: T.matmul(PS[4 + fc][:, :], slab[:, sl, 1, fc * 128:(fc + 1) * 128], rs, start=(tc == 0), stop=last_s),
                                     reads=[('slab', sl, 1), 'ob'], writes=[psk(4 + fc)], signal=(tc == 15 or fc == 3))
                        rk = [('rows', fp % 2), 'eo2048']
                        for fc in range(4):
                            if even:
                                S.op('pe', lambda: T.matmul(PS[fc][:, :], rows[0:1, fp % 2, 0, fc * 128:(fc + 1) * 128], eo2048[0:1, 0, :], start=False, stop=True), reads=rk, writes=[psk(fc)])
                            else:
                                S.op('pe', lambda: T.matmul(PS[4 + fc][:, :], rows[0:1, fp % 2, 1, fc * 128:(fc + 1) * 128], eo2048[0:1, 1, :], start=False, stop=True), reads=rk, writes=[psk(4 + fc)])
                        if fp == 0:
                            for tc in range(16):
                                S.op('pe', lambda: T.matmul(PS[4][:, :], nqs[:, tc, :], eb[:, tc, :], start=False, stop=False),
                                     reads=['nqs', 'eb'], writes=[psk(4)], signal=(tc == 15))
                            S.op('pe', lambda: T.matmul(PS[4][:, :], nqs[0:1, 0, :], eo2048[0:1, 0, :], start=False, stop=True), reads=['nqs', 'eo2048'], writes=[psk(4)])
                        for fc in range(4):
                            fcg = fp * 4 + fc
                            par = fcg % 2
                            S.op('act', lambda: A.activation(out=abo[:, par, 0, :], in_=PS[fc][:, :], func=AF.Copy, scale=wcol[:, fcg:fcg + 1]),
                                 reads=[psk(fc), 'wcol'], writes=[('abo', par, 0)])
                            S.op('act', lambda: A.activation(out=abo[:, par, 1, :], in_=PS[4 + fc][:, :], func=AF.Copy, scale=wcol[:, fcg:fcg + 1]),
                                 reads=[psk(4 + fc), 'wcol'], writes=[('abo', par, 1)])
                            S.dma('pool', AB_d[o, 0, fcg * 128:(fcg + 1) * 128, :], abo[:, par, 0, :], reads=[('abo', par, 0)], writes=[('AB', o)])
                            S.dma('pool', AB_d[o, 1, fcg * 128:(fcg + 1) * 128, :], abo[:, par, 1, :], reads=[('abo', par, 1)], writes=[('AB', o)])
                S.barrier()

        def phase_AB(l):
            with ExitStack() as es_:
                WA = es_.enter_context(nc.sbuf_tensor(UN("WA"), [128, 8, NWA * 128], BF16))
                for dc in range(8):
                    S.dma('pool', WA[:, dc, :], I['wa'][l, dc * 128:(dc + 1) * 128, :], writes=['WA'])
                load_gain(I['mix_norm'][l])
                for b in range(NBC):
                    xsrc = x_in[b] if l == 0 else xs[b]
                    with ExitStack() as es_:
                        ropec = es_.enter_context(nc.sbuf_tensor(UN("ropec"), [128, 2, 512], BF16))
                        ropes = es_.enter_context(nc.sbuf_tensor(UN("ropes"), [128, 2, 512], BF16))
                        qT = es_.enter_context(nc.sbuf_tensor(UN("qT"), [128, 4, L], BF16))
                        kTm = es_.enter_context(nc.sbuf_tensor(UN("kTm"), [128, 2, L], BF16))
                        vpad = es_.enter_context(nc.sbuf_tensor(UN("vpad"), [128, 2, 32, 128], BF16))
                        xt = es_.enter_context(nc.sbuf_tensor(UN("xt"), [128, 2, D], F32))
                        xn = es_.enter_context(nc.sbuf_tensor(UN("xn"), [128, 2, D], BF16))
                        hT2b = es_.enter_context(nc.sbuf_tensor(UN("hT"), [128, 2, 8, 512], BF16))
                        rawu = es_.enter_context(nc.sbuf_tensor(UN("rawu"), [128, 12, 514], BF16))
                        cw = es_.enter_context(nc.sbuf_tensor(UN("cw"), [128, 12, 3], F32))
                        cb = es_.enter_context(nc.sbuf_tensor(UN("cb"), [128, 12], F32))
                        ctmp = es_.enter_context(nc.sbuf_tensor(UN("ctmp"), [128, 2, 2, 512], F32))
                        uo = es_.enter_context(nc.sbuf_tensor(UN("uo"), [128, 2, 512], BF16))
                        sinkE = es_.enter_context(nc.sbuf_tensor(UN("sinkE"), [128, 4], F32))
                        PT = es_.enter_context(nc.sbuf_tensor(UN("PT"), [128, 2, 3, 512], BF16))
                        den = es_.enter_context(nc.sbuf_tensor(UN("den"), [128, 512], F32))
                        usb = es_.enter_context(nc.sbuf_tensor(UN("usb"), [128, 512], F32))
                        ao = es_.enter_context(nc.sbuf_tensor(UN("ao"), [128, 2, 512], BF16))
                        with nc.allow_non_contiguous_dma(reason="tiny"):
                            for k in range(3):
                                S.dma('sp', cw[:, :, k], I['cw'][l, k].rearrange("(j p) -> p j", p=128), writes=['cw'])
                            S.dma('sp', cb[:, :], I['cb'][l].rearrange("(j p) -> p j", p=128), writes=['cw'])
                            for kvh in range(2):
                                S.dma('sp', sinkE[kvh * 64:(kvh + 1) * 64, :], I['attn_sink'][l, kvh * 4:(kvh + 1) * 4].partition_broadcast(64), writes=['sinkE'])
                        S.op('act', lambda: A.activation(out=sinkE[:, :], in_=sinkE[:, :], func=AF.Exp), reads=['sinkE'], writes=['sinkE'])
                        S.op('dve', lambda: V.memset(kTm[:, :, :], 0.0), writes=['kTm'])
                        S.op('dve', lambda: V.memset(vpad[:, :, :, :], 0.0), writes=['vpad'])
                        S.op('dve', lambda: V.memset(rawu[:, :, :], 0.0), writes=[('rawu', j) for j in range(12)])

                        def conv_out(j, width, col0, par):
                            rk = ('rawu', j)
                            t1 = ctmp[:, par, 0, 0:width]
                            t2 = ctmp[:, par, 1, 0:width]
                            S.op('act', lambda: A.activation(out=t1, in_=rawu[:, j, 1:1 + width], func=AF.Identity, bias=cb[:, j:j + 1], scale=cw[:, j, 1:2]),
                                 reads=[rk, 'cw'], writes=[('ct', par, 0)])
                            S.op('dve', lambda: V.scalar_tensor_tensor(out=t2, in0=rawu[:, j, 0:width], scalar=cw[:, j, 0:1], in1=t1, op0=ALU.mult, op1=ALU.add),
                                 reads=[rk, 'cw', ('ct', par, 0)], writes=[('ct', par, 1)])
                            S.op('dve', lambda: V.scalar_tensor_tensor(out=uo[:, par, 0:width], in0=rawu[:, j, 2:2 + width], scalar=cw[:, j, 2:3], in1=t2, op0=ALU.mult, op1=ALU.add),
                                 reads=[rk, 'cw', ('ct', par, 1)], writes=[('uo', par)])
                            with nc.allow_non_contiguous_dma(reason="edge"):
                                S.dma('pool', U_d[b, j, :, col0:col0 + width], uo[:, par, 0:width], reads=[('uo', par)], writes=[('U', b, j)])

                        def front(n):
                            for t2 in range(2):
                                items = []
                                for tt in (2 * t2, 2 * t2 + 1):
                                    r0 = n * 512 + tt * 128
                                    S.dma('sp', xt[:, tt % 2, :], xsrc[r0:r0 + 128, :], writes=[('xt', tt % 2)])
                                    items.append((xt[:, tt % 2, :], ('xt', tt % 2), xn[:, tt % 2, :], ('xn', tt % 2), hT2b[:, n % 2, :, tt * 128:(tt + 1) * 128], ('hT', n % 2, tt), tt % 2))
                                norm_T_batch(items)

                        front(0)
                        for n in range(8):
                            tok0 = n * 512
                            hT = hT2b[:, n % 2]
                            S.dma('sp', ropec[:, n % 2, :], I['ropec'][:, tok0:tok0 + 512], writes=[('rope', n % 2)])
                            S.dma('sp', ropes[:, n % 2, :], I['ropes'][:, tok0:tok0 + 512], writes=[('rope', n % 2)])
                            hk = [('hT', n % 2, tt) for tt in range(4)]
                            for g in range(5):
                                ca, cs = (g, 4 + g) if g < 4 else (8, 9)
                                mm_group(2, [(WA[:, dc, ca * 128:(ca + 1) * 128], hT[:, dc, :]) for dc in range(8)], hk + ['WA'])
                                mm_group(3, [(WA[:, dc, cs * 128:(cs + 1) * 128], hT[:, dc, :]) for dc in range(8)], hk + ['WA'])
                                par = g % 2
                                t1 = ctmp[:, par, 0, :]
                                t2 = ctmp[:, par, 1, :]
                                S.op('dve', lambda: V.tensor_tensor(out=t1, in0=PS[2][:, :], in1=ropec[:, n % 2, :], op=ALU.mult), reads=[psk(2), ('rope', n % 2)], writes=[('ct', par, 0)])
                                S.op('dve', lambda: V.tensor_tensor(out=t2, in0=PS[3][:, :], in1=ropes[:, n % 2, :], op=ALU.mult), reads=[psk(3), ('rope', n % 2)], writes=[('ct', par, 1)])
                                if g < 4:
                                    S.op('dve', lambda: V.tensor_tensor(out=qT[:, g, tok0:tok0 + 512], in0=t1, in1=t2, op=ALU.add),
                                         reads=[('ct', par, 0), ('ct', par, 1)], writes=['qT'])
                                else:
                                    for kvh in range(2):
                                        sl = slice(kvh * 64, (kvh + 1) * 64)
                                        S.op('dve', lambda: V.tensor_tensor(out=kTm[sl, kvh, tok0:tok0 + 512], in0=ctmp[sl, par, 0, :], in1=ctmp[sl, par, 1, :], op=ALU.add),
                                             reads=[('ct', par, 0), ('ct', par, 1)], writes=['kTm'])
                            for tt in range(4):
                                for dc in range(8):
                                    S.op('pe', lambda: T.matmul(PS[4][:, tt * 128:(tt + 1) * 128], hT[:, dc, tt * 128:(tt + 1) * 128], WA[:, dc, 10 * 128:11 * 128], start=(dc == 0), stop=(dc == 7)),
                                         reads=hk + ['WA'], writes=[psk(4)], signal=(tt == 3 and dc == 7))
                            for kvh in range(2):
                                S.op('act', lambda: A.copy(out=vpad[:, kvh, n * 4:(n + 1) * 4, kvh * 64:(kvh + 1) * 64],
                                                           in_=PS[4][:, :].rearrange("p (t c) -> p t c", t=4)[:, :, kvh * 64:(kvh + 1) * 64]),
                                     reads=[psk(4)], writes=['vpad'])
                            if n + 1 < 8:
                                front(n + 1)
                            for j in range(12):
                                bank = (5, 6, 7, 2, 3, 4)[j % 6]
                                mm_group(bank, [(WA[:, dc, (11 + j) * 128:(12 + j) * 128], hT[:, dc, :]) for dc in range(8)], hk + ['WA'])
                                S.op('act', lambda: A.copy(out=rawu[:, j, 2:514], in_=PS[bank][:, :]), reads=[psk(bank)], writes=[('rawu', j)])
                                conv_out(j, 512, tok0, j % 2)
                                S.op('act', lambda: A.copy(out=rawu[:, j, 0:2], in_=rawu[:, j, 512:514]), reads=[('rawu', j)], writes=[('rawu', j)])
                        for j in range(12):
                            S.op('dve', lambda: V.memset(rawu[:, j, 2:3], 0.0), reads=[('rawu', j)], writes=[('rawu', j)])
                            conv_out(j, 1, L, j % 2)

                        units = [(i, kvh) for i in range(32) for kvh in range(2)]

                        def blocks_of(i):
                            return [j for j in (i - 1, i, i + 1) if 0 <= j < 32]

                        def scores(ui):
                            i, kvh = units[ui]
                            for jj, j in enumerate(blocks_of(i)):
                                bank = (ui % 2) * 3 + jj
                                S.op('pe', lambda: T.matmul(PS[bank][:, :], kTm[:, kvh, j * 128:(j + 1) * 128], qT[:, :, i * 128:(i + 1) * 128], start=True, stop=True),
                                     reads=['kTm', 'qT'], writes=[psk(bank)])

                        scores(0)
                        for ui, (i, kvh) in enumerate(units):
                            if ui + 1 < len(units):
                                scores(ui + 1)
                            blocks = blocks_of(i)
                            for jj, j in enumerate(blocks):
                                bank = (ui % 2) * 3 + jj
                                S.op('act', lambda: A.activation(out=PT[:, kvh, jj, :], in_=PS[bank][:, :], func=AF.Exp, scale=0.125),
                                     reads=[psk(bank)], writes=[('PT', kvh, jj)])
                                if j != i:
                                    mk = mprev if j < i else mnext
                                    pv = PT[:, kvh, jj, :].rearrange("p (g t) -> p g t", g=4)
                                    S.op('dve', lambda: V.tensor_tensor(out=pv, in0=pv, in1=mk[:, :].unsqueeze(1).broadcast_to([128, 4, 128]), op=ALU.mult),
                                         reads=[('PT', kvh, jj), 'mprev', 'mnext'], writes=[('PT', kvh, jj)])
                            nb_ = len(blocks)
                            for jj, j in enumerate(blocks):
                                first = (kvh == 0 and jj == 0)
                                lastm = (kvh == 1 and jj == nb_ - 1)
                                S.op('pe', lambda: T.matmul(PS[6][:, :], vpad[:, kvh, j, :], PT[:, kvh, jj, :], start=first, stop=lastm),
                                     reads=['vpad', ('PT', kvh, jj)], writes=[psk(6)], signal=lastm)
                                S.op('pe', lambda: T.matmul(PS[7][:, :], onesm[:, kvh, :], PT[:, kvh, jj, :], start=first, stop=lastm),
                                     reads=['onesm', ('PT', kvh, jj)], writes=[psk(7)], signal=(lastm or jj == nb_ - 1))
                            if kvh == 1:
                                S.op('act', lambda: A.copy(out=usb[:, :], in_=PS[6][:, :]), reads=[psk(6)], writes=['usb'])
                                S.op('act', lambda: A.copy(out=den[:, :], in_=PS[7][:, :]), reads=[psk(7)], writes=['den'])
                                S.op('dve', lambda: V.tensor_tensor(out=den[:, :].rearrange("p (g t) -> p g t", g=4), in0=den[:, :].rearrange("p (g t) -> p g t", g=4),
                                                                    in1=sinkE[:, :].unsqueeze(2).broadcast_to([128, 4, 128]), op=ALU.add),
                                     reads=['den', 'sinkE'], writes=['den'])
                                S.op('act', lambda: A.activation(out=den[:, :], in_=den[:, :], func=AF.Ln), reads=['den'], writes=['den'])
                                S.op('act', lambda: A.activation(out=den[:, :], in_=den[:, :], func=AF.Exp, scale=-1.0), reads=['den'], writes=['den'])
                                S.op('dve', lambda: V.tensor_tensor(out=ao[:, i % 2, :], in0=usb[:, :], in1=den[:, :], op=ALU.mult), reads=['usb', 'den'], writes=[('ao', i % 2)])
                                S.dma('pool', aT_d[b, :, :, i * 128:(i + 1) * 128], ao[:, i % 2, :].rearrange("p (g t) -> p g t", g=4), reads=[('ao', i % 2)], writes=[('aT', b)])
                        S.barrier()

        def phase_H(l):
            for b in range(NBC):
                with ExitStack() as es_:
                    vtm = es_.enter_context(nc.sbuf_tensor(UN("vtm"), [128, 32, 512], BF16))
                    Pr = es_.enter_context(nc.sbuf_tensor(UN("Pr"), [128, 32, 512], BF16))
                    Pq = es_.enter_context(nc.sbuf_tensor(UN("Pq"), [128, 32, 512], BF16))
                    hslab = es_.enter_context(nc.sbuf_tensor(UN("hslab"), [128, 6, 1024], BF16))
                    abt = es_.enter_context(nc.sbuf_tensor(UN("abt"), [128, 2, 4, 2, 512], BF16))
                    htmp = es_.enter_context(nc.sbuf_tensor(UN("htmp"), [128, 2, 2, 512], F32))
                    yo = es_.enter_context(nc.sbuf_tensor(UN("yo"), [128, 2, 512], BF16))
                    yo2 = es_.enter_context(nc.sbuf_tensor(UN("yo2"), [128, 2, 512], BF16))
                    zc = es_.enter_context(nc.sbuf_tensor(UN("zc"), [128, 4, 2, 512], F32))
                    gl2 = es_.enter_context(nc.sbuf_tensor(UN("gl2"), [128, 2, 4, 512], BF16))
                    ld2 = es_.enter_context(nc.sbuf_tensor(UN("ld2"), [128, 2, 512], BF16))
                    gl = es_.enter_context(nc.sbuf_tensor(UN("gl"), [128, 2, 4, 512], BF16))
                    ld = es_.enter_context(nc.sbuf_tensor(UN("ld"), [128, 2, 512], BF16))
                    def to_token_major(src_fn, srck):
                        for tg in range(4):
                            t0 = tg * 512
                            for cc in range(4):
                                par = (tg * 4 + cc) % 2
                                S.dma('sp', ld[:, par, :], src_fn(cc, t0), reads=[srck], writes=[('ld', par)])
                                S.dma('sp', ld2[:, par, :], src_fn(cc, 3584 - t0), reads=[srck], writes=[('ld2', par)])
                                S.op('dve', lambda: V.tensor_tensor(out=yo[:, par, :], in0=ld[:, par, :], in1=ld2[:, par, :][:, ::-1], op=ALU.add),
                                     reads=[('ld', par), ('ld2', par)], writes=[('yo', par)])
                                S.op('dve', lambda: V.tensor_tensor(out=yo2[:, par, :], in0=ld[:, par, :], in1=ld2[:, par, :][:, ::-1], op=ALU.subtract),
                                     reads=[('ld', par), ('ld2', par)], writes=[('yo2', par)])
                                for k in range(4):
                                    pbp = PS[k][:, :].bitcast(BF16)
                                    pbm = PS[4 + k][:, :].bitcast(BF16)
                                    S.op('pe', lambda: T.transpose(pbp[:, cc * 128:(cc + 1) * 128], yo[:, par, k * 128:(k + 1) * 128], identb[:, :]),
                                         reads=[('yo', par), 'identb'], writes=[psk(k)], signal=True)
                                    S.op('pe', lambda: T.transpose(pbm[:, cc * 128:(cc + 1) * 128], yo2[:, par, k * 128:(k + 1) * 128], identb[:, :]),
                                         reads=[('yo2', par), 'identb'], writes=[psk(4 + k)], signal=True)
                            for k in range(4):
                                S.op('act', lambda: A.copy(out=vtm[:, tg * 4 + k, :], in_=PS[k][:, :].bitcast(BF16)[:, 0:512]), reads=[psk(k)], writes=['vtm'])
                                S.op('dve', lambda: V.tensor_copy(out=vtm[:, 16 + tg * 4 + k, :], in_=PS[4 + k][:, :].bitcast(BF16)[:, 0:512]), reads=[psk(4 + k)], writes=['vtm'])

                    def forward(o):
                        for fp in range(8):
                            even = fp < 4
                            pp = fp % 2
                            for fc in range(4):
                                fcg = fp * 4 + fc
                                S.dma('pool', abt[:, pp, fc, 0, :], AB_d[o, 0, fcg * 128:(fcg + 1) * 128, :], reads=[('AB', o)], writes=[('abt', pp, fc, 0)])
                                S.dma('pool', abt[:, pp, fc, 1, :], AB_d[o, 1, fcg * 128:(fcg + 1) * 128, :], reads=[('AB', o)], writes=[('abt', pp, fc, 1)])
                            for tc in range(16):
                                sl = (fp * 16 + tc) % 6
                                S.dma('sp', hslab[:, sl, 0:512], I['dfcf'][tc * 128:(tc + 1) * 128, fp * 512:(fp + 1) * 512], writes=[('hs', sl, 0)])
                                S.dma('sp', hslab[:, sl, 512:1024], I['dfsf'][tc * 128:(tc + 1) * 128, fp * 512:(fp + 1) * 512], writes=[('hs', sl, 1)])
                                rc = vtm[:, tc if even else 16 + tc, :]
                                rs = vtm[:, 16 + tc if even else tc, :]
                                for fc in range(4):
                                    S.op('pe', lambda: T.matmul(PS[fc][:, :], hslab[:, sl, fc * 128:(fc + 1) * 128], rc, start=(tc == 0), stop=(tc == 15)),
                                         reads=[('hs', sl, 0), 'vtm'], writes=[psk(fc)], signal=(tc == 15))
                                    S.op('pe', lambda: T.matmul(PS[4 + fc][:, :], hslab[:, sl, 512 + fc * 128:512 + (fc + 1) * 128], rs, start=(tc == 0), stop=(tc == 15)),
                                         reads=[('hs', sl, 1), 'vtm'], writes=[psk(4 + fc)], signal=(tc == 15 or fc == 3))
                            for fc in range(4):
                                fcg = fp * 4 + fc
                                par = fcg % 2
                                Zr, Zs = zc[:, fc, 0, :], zc[:, fc, 1, :]
                                S.op('act', lambda: A.copy(out=Zr, in_=PS[fc][:, :]), reads=[psk(fc)], writes=[('zc', fc, 0)])
                                S.op('act', lambda: A.copy(out=Zs, in_=PS[4 + fc][:, :]), reads=[psk(4 + fc)], writes=[('zc', fc, 1)])
                                Am, Bm = abt[:, pp, fc, 0, :], abt[:, pp, fc, 1, :]
                                t1, t2 = htmp[:, par, 0, :], htmp[:, par, 1, :]
                                kz = [('zc', fc, 0), ('zc', fc, 1), ('abt', pp, fc, 0), ('abt', pp, fc, 1)]
                                S.op('dve', lambda: V.tensor_tensor(out=t1, in0=Zr, in1=Am, op=ALU.mult), reads=kz, writes=[('ht', par, 0)])
                                S.op('dve', lambda: V.tensor_tensor(out=t2, in0=Zs, in1=Bm, op=ALU.mult), reads=kz, writes=[('ht', par, 1)])
                                S.op('dve', lambda: V.tensor_tensor(out=Pr[:, fcg, :], in0=t1, in1=t2, op=ALU.subtract), reads=[('ht', par, 0), ('ht', par, 1)], writes=['Pr'])
                                S.op('dve', lambda: V.tensor_tensor(out=t1, in0=Zr, in1=Bm, op=ALU.mult), reads=kz, writes=[('ht', par, 0)])
                                S.op('dve', lambda: V.tensor_tensor(out=t2, in0=Zs, in1=Am, op=ALU.mult), reads=kz, writes=[('ht', par, 1)])
                                S.op('dve', lambda: V.tensor_tensor(out=Pq[:, fcg, :], in0=t1, in1=t2, op=ALU.add), reads=[('ht', par, 0), ('ht', par, 1)], writes=['Pq'])
                                if fcg == 0:
                                    S.op('dve', lambda: V.tensor_tensor(out=Pr[0:1, 0, :], in0=Zr[0:1, :], in1=Am[0:1, :], op=ALU.mult), reads=kz + ['Pr'], writes=['Pr'])
                                    S.op('dve', lambda: V.tensor_tensor(out=Pq[0:1, 0, :], in0=Zs[0:1, :], in1=Bm[0:1, :], op=ALU.mult), reads=kz + ['Pq'], writes=['Pq'])

                    def inverse(gate_j, dst_fn, dstk):
                        for tp in range(4):
                            t0 = tp * 512
                            pp = tp % 2
                            for cc in range(4):
                                gk = ('U', b, gate_j * 4 + cc)
                                S.dma('pool', gl[:, pp, cc, :], U_d[b, gate_j * 4 + cc, :, 1 + t0:1 + t0 + 512], reads=[gk], writes=[('gl', pp, cc)])
                                S.dma('pool', gl2[:, pp, cc, :], U_d[b, gate_j * 4 + cc, :, 1 + (3584 - t0):1 + (3584 - t0) + 512], reads=[gk], writes=[('gl2', pp, cc)])
                            for k in range(32):
                                sl = (tp * 32 + k) % 6
                                S.dma('sp', hslab[:, sl, 0:512], I['dfci'][k * 128:(k + 1) * 128, t0:t0 + 512], writes=[('hs', sl, 0)])
                                S.dma('sp', hslab[:, sl, 512:1024], I['dfsi'][k * 128:(k + 1) * 128, t0:t0 + 512], writes=[('hs', sl, 1)])
                                even = k < 16
                                Cs, Ss = hslab[:, sl, 0:512], hslab[:, sl, 512:1024]
                                for cc in range(4):
                                    Al, Ar = (Pr, Cs) if even else (Pq, Ss)
                                    Bl, Br = (Pq, Ss) if even else (Pr, Cs)
                                    rk = [('hs', sl, 0), ('hs', sl, 1), 'Pr', 'Pq']
                                    S.op('pe', lambda: T.matmul(PS[cc * 2][:, :], Al[:, k, cc * 128:(cc + 1) * 128], Ar, start=(k == 0), stop=(k == 31)),
                                         reads=rk, writes=[psk(cc * 2)], signal=(k == 31))
                                    S.op('pe', lambda: T.matmul(PS[cc * 2 + 1][:, :], Bl[:, k, cc * 128:(cc + 1) * 128], Br, start=(k == 0), stop=(k == 31)),
                                         reads=rk, writes=[psk(cc * 2 + 1)], signal=(k == 31 or cc == 3))
                            for cc in range(4):
                                par = cc % 2
                                u0 = 3584 - t0
                                gk = ('U', b, gate_j * 4 + cc)
                                Bs, Sm = htmp[:, par, 0, :], htmp[:, par, 1, :]
                                Ac, Bc = zc[:, cc, 0, :], zc[:, cc, 1, :]
                                S.op('act', lambda: A.copy(out=Ac, in_=PS[cc * 2][:, :]), reads=[psk(cc * 2)], writes=[('zc', cc, 0)])
                                S.op('act', lambda: A.copy(out=Bc, in_=PS[cc * 2 + 1][:, :]), reads=[psk(cc * 2 + 1)], writes=[('zc', cc, 1)])
                                S.op('dve', lambda: V.tensor_tensor(out=Sm, in0=Ac, in1=Bc, op=ALU.add), reads=[('zc', cc, 0), ('zc', cc, 1)], writes=[('ht', par, 1)])
                                S.op('dve', lambda: V.tensor_tensor(out=Bs, in0=Ac, in1=Bc, op=ALU.subtract), reads=[('zc', cc, 0), ('zc', cc, 1)], writes=[('ht', par, 0)])
                                S.op('dve', lambda: V.tensor_tensor(out=yo[:, par, :], in0=Sm, in1=gl[:, pp, cc, :], op=ALU.mult), reads=[('ht', par, 1), ('gl', pp, cc)], writes=[('yo', par)])
                                S.op('dve', lambda: V.tensor_tensor(out=yo2[:, par, :], in0=Bs[:, ::-1], in1=gl2[:, pp, cc, :], op=ALU.mult), reads=[('ht', par, 0), ('gl2', pp, cc)], writes=[('yo2', par)])
                                S.dma('pool', dst_fn(cc, t0), yo[:, par, :], reads=[('yo', par)], writes=[dstk])
                                S.dma('pool', dst_fn(cc, u0), yo2[:, par, :], reads=[('yo2', par)], writes=[dstk])

                    to_token_major(lambda cc, t0: U_d[b, 8 + cc, :, 1 + t0:1 + t0 + 512], ('U', b, 8))
                    forward(0)
                    inverse(0, lambda cc, t0: ZZ_d[cc, :, t0:t0 + 512], 'ZZ')
                    to_token_major(lambda cc, t0: ZZ_d[cc, :, t0:t0 + 512], 'ZZ')
                    forward(1)
                    inverse(1, lambda cc, t0: yT_d[b, :, cc, t0:t0 + 512], ('yT', b))
                    S.barrier()

        def phase_O(l):
            with ExitStack() as es_:
                WO = es_.enter_context(nc.sbuf_tensor(UN("WO"), [128, 8, D], BF16))
                wst = es_.enter_context(nc.sbuf_tensor(UN("wst"), [128, 2, D], F32))
                gcolo = es_.enter_context(nc.sbuf_tensor(UN("gcolo"), [128, 8], F32))
                xo = es_.enter_context(nc.sbuf_tensor(UN("xo"), [128, 2, D], F32))
                at = es_.enter_context(nc.sbuf_tensor(UN("at"), [128, 2, 2, 4, 128], BF16))
                sq = es_.enter_context(nc.sbuf_tensor(UN("sq"), [128, 2, 2, 4, 128], BF16))
                rs = es_.enter_context(nc.sbuf_tensor(UN("rs"), [128, 2, 2], F32))
                with nc.allow_non_contiguous_dma(reason="tiny"):
                    S.dma('sp', gcolo[:, 0:4], I['ga'][l].rearrange("(k p) -> p k", p=128), writes=['gcolo'])
                    S.dma('sp', gcolo[:, 4:8], I['gy'][l].rearrange("(k p) -> p k", p=128), writes=['gcolo'])
                for k in range(8):
                    S.dma('sp', wst[:, k % 2, :], I['w_out'][l, k * 128:(k + 1) * 128, :], writes=[('wst', k % 2)])
                    S.op('dve', lambda: V.tensor_scalar(out=WO[:, k, :], in0=wst[:, k % 2, :], scalar1=gcolo[:, k:k + 1], scalar2=None, op0=ALU.mult),
                         reads=[('wst', k % 2), 'gcolo'], writes=['WO'])
                for b in range(NBC):
                    xsrc = x_in[b] if l == 0 else xs[b]
                    for i in range(32):
                        par = i % 2
                        r0 = i * 128
                        S.dma('sp', xo[:, par, :], xsrc[r0:r0 + 128, :], writes=[('xo', par)])
                        S.dma('sp', at[:, par, 0, :, :], aT_d[b, :, :, r0:r0 + 128], writes=[('at', par, 0)])
                        S.dma('sp', at[:, par, 1, :, :], yT_d[b, :, :, r0:r0 + 128], writes=[('at', par, 1)])
                        for w in range(2):
                            S.op('act', lambda: A.activation(out=sq[:, par, w, :, :], in_=at[:, par, w, :, :], func=AF.Square), reads=[('at', par, w)], writes=[('sq', par, w)])
                            for g in range(4):
                                S.op('pe', lambda: T.matmul(PS[4 + w][:, 0:1], sq[:, par, w, g, :], onesb[:, 0:1], start=(g == 0), stop=(g == 3)),
                                     reads=[('sq', par, w), 'onesb'], writes=[psk(4 + w)], signal=(g == 3))
                            S.op('act', lambda: A.activation(out=rs[:, par, w:w + 1], in_=PS[4 + w][:, 0:1], func=AF.Ln, bias=epsc[:, 0:1], scale=1.0 / 512),
                                 reads=[psk(4 + w)], writes=[('rs', par, w)])
                            S.op('act', lambda: A.activation(out=rs[:, par, w:w + 1], in_=rs[:, par, w:w + 1], func=AF.Exp, scale=-0.5), reads=[('rs', par, w)], writes=[('rs', par, w)])
                            for half in range(2):
                                bank = w * 2 + half
                                mm_group(bank, [(at[:, par, w, g, :], WO[:, w * 4 + g, half * 512:(half + 1) * 512]) for g in range(4)], [('at', par, w), 'WO'])
                                S.op('dve', lambda: V.scalar_tensor_tensor(out=xo[:, par, half * 512:(half + 1) * 512], in0=PS[bank][:, :], scalar=rs[:, par, w:w + 1],
                                                                           in1=xo[:, par, half * 512:(half + 1) * 512], op0=ALU.mult, op1=ALU.add),
                                     reads=[psk(bank), ('rs', par, w), ('xo', par)], writes=[('xo', par)])
                        S.dma('pool', xs[b, r0:r0 + 128, :], xo[:, par, :], reads=[('xo', par)], writes=[('xs', b)])
                S.barrier()

        def phase_XF(l):
            last = (l == n_layers - 1)
            moe = (l % 2 == 1)
            nfc = (DFE if moe else DFF) // 128
            with ExitStack() as es_:
                XQ = es_.enter_context(nc.sbuf_tensor(UN("XQ"), [128, 8, 512], BF16))
                XO = es_.enter_context(nc.sbuf_tensor(UN("XO"), [128, 4, D], BF16))
                gx = es_.enter_context(nc.sbuf_tensor(UN("gx"), [128, D], F32))
                gf = es_.enter_context(nc.sbuf_tensor(UN("gf"), [128, D], F32))
                xt2 = es_.enter_context(nc.sbuf_tensor(UN("xt2"), [128, 4, D], F32))
                xn2 = es_.enter_context(nc.sbuf_tensor(UN("xn2"), [128, 4, D], BF16))
                hT2 = es_.enter_context(nc.sbuf_tensor(UN("hT2"), [128, 8, 512], BF16))
                qx = es_.enter_context(nc.sbuf_tensor(UN("qx"), [128, 4, 512], BF16))
                PTx = es_.enter_context(nc.sbuf_tensor(UN("PTx"), [128, 2, 2, 512], BF16))
                ox = es_.enter_context(nc.sbuf_tensor(UN("ox"), [128, 4, 512], BF16))
                rdn = es_.enter_context(nc.sbuf_tensor(UN("rdn"), [128, 2, 512], F32))
                wgs = es_.enter_context(nc.sbuf_tensor(UN("wgs"), [128, 3, 8, 128], BF16))
                wus = es_.enter_context(nc.sbuf_tensor(UN("wus"), [128, 3, 8, 128], BF16))
                wds = es_.enter_context(nc.sbuf_tensor(UN("wds"), [128, 3, D], BF16))
                actT = es_.enter_context(nc.sbuf_tensor(UN("actT"), [128, 28, 512], BF16))
                sg = es_.enter_context(nc.sbuf_tensor(UN("sg"), [128, 2, 512], F32))
                hn32 = es_.enter_context(nc.sbuf_tensor(UN("hn32"), [128, D], F32))
                hT32 = es_.enter_context(nc.sbuf_tensor(UN("hT32"), [128, 8, 128], F32))
                identf = es_.enter_context(nc.sbuf_tensor(UN("identf"), [128, 128], F32))
                wr = es_.enter_context(nc.sbuf_tensor(UN("wr"), [128, 8, NE], F32))
                lg = es_.enter_context(nc.sbuf_tensor(UN("lg"), [128, 4, NE], F32))
                m8 = es_.enter_context(nc.sbuf_tensor(UN("m8"), [128, 4, 8], F32))
                gw = es_.enter_context(nc.sbuf_tensor(UN("gw"), [128, 4, NE], F32))
                gtmp = es_.enter_context(nc.sbuf_tensor(UN("gtmp"), [128, 4, NE], F32))
                yfin = es_.enter_context(nc.sbuf_tensor(UN("yfin"), [128, 2, D], F32))
                with ExitStack() as es_in:
                    XK = es_in.enter_context(nc.sbuf_tensor(UN("XK"), [128, 8, 512], BF16))
                    XV = es_in.enter_context(nc.sbuf_tensor(UN("XV"), [128, 8, 512], BF16))
                    for nm, tl in (('xw_q', XQ), ('xw_k', XK), ('xw_v', XV)):
                        S.dma('pool', tl[:, :, :], I[nm][l].rearrange("(k p) n -> p k n", p=128), writes=[nm])
                    S.dma('pool', XO[:, :, :], I['xw_o'][l].rearrange("(k p) n -> p k n", p=128), writes=['xw_o'])
                    S.op('act', lambda: A.copy(out=identf[:, :], in_=identb[:, :]), reads=['identb'], writes=['identf'])
                    with nc.allow_non_contiguous_dma(reason="small"):
                        S.dma('sp', wr[:, :, :], I['moe_router'].rearrange("(k p) e -> p k e", p=128), writes=['wr'])
                        S.dma('sp', gx[:, :], I['xattn_norm'][l].partition_broadcast(128), writes=['gx'])
                        S.dma('sp', gf[:, :], I['ffn_norm'][l].partition_broadcast(128), writes=['gf'])
                        if last:
                            S.dma('sp', gbc[:, :], I['final_norm'].partition_broadcast(128), writes=['gbc'])
                    for h in range(4):
                        mm_group(h % 2, [(XK[:, dc, h * 128:(h + 1) * 128], mT[:, dc, :]) for dc in range(8)], ['xw_k'])
                        S.op('act', lambda: A.copy(out=KmT[:, h, :], in_=PS[h % 2][:, :]), reads=[psk(h % 2)], writes=['KmT'])
                    for bj in range(NBC * 2):
                        mm_group(2 + bj % 2, [(mT[:, dc, bj * 128:(bj + 1) * 128], XV[:, dc, :]) for dc in range(8)], ['xw_v'])
                        S.op('act', lambda: A.copy(out=Vm[:, bj, :], in_=PS[2 + bj % 2][:, :]), reads=[psk(2 + bj % 2)], writes=['Vm'])
                    S.barrier()

                wctr = [0]
                Mcat = es_.enter_context(nc.sbuf_tensor(UN("Mcat"), [128, 64, 16], F32))
                Wts = es_.enter_context(nc.sbuf_tensor(UN("Wts"), [128, 64, 2], F32))
                sloti = es_.enter_context(nc.sbuf_tensor(UN("sloti"), [128, 128], mybir.dt.int32))
                eblki = es_.enter_context(nc.sbuf_tensor(UN("eblki"), [128, NBLK], mybir.dt.int32))

                def moe_sparse(last):
                    with ExitStack() as es2:
                        sb = lambda nm, shp, dt: es2.enter_context(nc.sbuf_tensor(UN(nm), shp, dt))
                        ltb = sb("ltb", [128, 128], BF16); slt = sb("slt", [64, 65], BF16)
                        thr32 = sb("thr32", [128, 32], F32); thr40 = sb("thr40", [128, NBLK], F32)
                        Mb = sb("Mb", [128, 64, 16], BF16); Mke = sb("Mke", [128, 16, 64], BF16)
                        within = sb("within", [128, 64, 16], F32)
                        totTs = sb("totTs", [64, 16], F32); totB = sb("totB", [64, 16, 128], BF16)
                        offs = sb("offs", [128, 16, 65], F32)
                        nn = sb("nn", [128, 8], F32); nb = sb("nb", [128, 8], F32); base = sb("base", [128, 9], F32)
                        cK = sb("cK", [128, 16], F32); cmp = sb("cmp", [128, NBLK], F32); eb = sb("ebf", [128, NBLK], F32)
                        sv = sb("sv", [128, 64, 16], F32); slotf = sb("slotf", [128, 128], F32)
                        zb = actT[:, 0:4, :].rearrange("p (n a) b -> p n (a b)", n=2)
                        yg = sb("yg", [128, 2, 2, D], F32)
                        rowb = sb("rowb", [128, DFE // 128], F32); widxf = sb("widxf", [128, 2, DFE // 128], F32); widxi = sb("widxi", [128, 2, DFE // 128], mybir.dt.int32)
                        S.dma('sp', rowb[:, :], I['rowbase'], writes=['rowb'])
                        S.dma('sp', ltb[:, :], I['ltb'], writes=['ltb'])
                        S.dma('sp', slt[:, :], I['slt65'], writes=['slt'])
                        S.dma('sp', thr32[:, :], I['thr32'], writes=['thr'])
                        S.dma('sp', thr40[:, :], I['thr40'], writes=['thr'])
                        S.op('dve', lambda: V.memset(zb[:, :, :], 0.0), writes=['zb'])
                        Gv = G_d.rearrange("(n p) d -> p n d", p=128)
                        for k in range(NBLK * 4 // 2):
                            S.dma('sp', Gv[:, k * 2:(k + 1) * 2, :], zb[:, :, :], reads=['zb'], writes=[('Gz', k)])
                        S.op('dve', lambda: V.tensor_copy(out=Mb[:, :, :], in_=Mcat[:, :, :]), reads=['Mcat'], writes=['Mb'])
                        S.op('dve', lambda: V.tensor_copy(out=Mke[:, :, :], in_=Mcat[:, :, :].rearrange("p t k -> p k t")), reads=['Mcat'], writes=['Mke'])
                        Mb2 = Mb[:, :, :].rearrange("p t k -> p (t k)")
                        for hh in range(2):
                            S.op('pe', lambda: T.matmul(PS[hh][:, :], ltb[:, :], Mb2[:, hh * 512:(hh + 1) * 512], start=True, stop=True), reads=['ltb', 'Mb'], writes=[psk(hh)])
                            S.op('dve', lambda: V.tensor_copy(out=within[:, :, :].rearrange("p t k -> p (t k)")[:, hh * 512:(hh + 1) * 512], in_=PS[hh][:, :]), reads=[psk(hh)], writes=['within'])
                        for ke in range(16):
                            S.op('pe', lambda: T.matmul(PS[2][0:64, ke:ke + 1], Mke[:, ke, :], onesb[:, 0:1], start=True, stop=True), reads=['Mke', 'onesb'], writes=[psk(2)], signal=(ke == 15))
                        S.op('dve', lambda: V.tensor_copy(out=totTs[:, :], in_=PS[2][0:64, 0:16]), reads=[psk(2)], writes=['totTs'])
                        S.op('dve', lambda: V.tensor_copy(out=totB[:, :, :], in_=totTs[:, :].unsqueeze(2).broadcast_to([64, 16, 128])), reads=['totTs'], writes=['totB'])
                        for ke in range(16):
                            bank, col = 3 + ke // 6, (ke % 6) * 65
                            S.op('pe', lambda: T.matmul(PS[bank][:, col:col + 65], totB[:, ke, :], slt[:, :], start=True, stop=True), reads=['totB', 'slt'], writes=[psk(bank)], signal=True)
                        for g3 in range(3):
                            nk = 6 if g3 < 2 else 4
                            S.op('dve', lambda: V.tensor_copy(out=offs[:, g3 * 6:g3 * 6 + nk, :], in_=PS[3 + g3][:, 0:nk * 65].rearrange("p (k t) -> p k t", k=nk)), reads=[psk(3 + g3)], writes=['offs'])
                        S.op('dve', lambda: V.tensor_tensor(out=nn[:, :], in0=offs[:, 0:8, 64], in1=offs[:, 8:16, 64], op=ALU.add), reads=['offs'], writes=['nn'])
                        for e in range(NE):
                            S.op('dve', lambda: V.tensor_scalar(out=cmp[:, 0:32], in0=thr32[:, :], scalar1=nn[:, e:e + 1], scalar2=None, op0=ALU.is_lt), reads=['thr', 'nn', 'cmp'], writes=['cmp'])
                            S.op('dve', lambda: V.reduce_sum(out=nb[:, e:e + 1], in_=cmp[:, 0:32], axis=mybir.AxisListType.X), reads=['cmp'], writes=['nb'])
                        S.op('dve', lambda: V.memset(base[:, 0:1], 0.0), writes=['base'])
                        for e in range(NE):
                            S.op('dve', lambda: V.scalar_tensor_tensor(out=base[:, e + 1:e + 2], in0=nb[:, e:e + 1], scalar=512.0, in1=base[:, e:e + 1], op0=ALU.mult, op1=ALU.add), reads=['nb', 'base'], writes=['base'])
                        S.op('dve', lambda: V.tensor_copy(out=cK[:, 0:8], in_=base[:, 0:8]), reads=['base'], writes=['cK'])
                        S.op('dve', lambda: V.tensor_tensor(out=cK[:, 8:16], in0=base[:, 0:8], in1=offs[:, 0:8, 64], op=ALU.add), reads=['base', 'offs'], writes=['cK'])
                        S.op('dve', lambda: V.tensor_tensor(out=sv[:, :, :], in0=within[:, :, :], in1=offs[:, :, 0:64].rearrange("p k t -> p t k"), op=ALU.add), reads=['within', 'offs'], writes=['sv'])
                        S.op('dve', lambda: V.tensor_tensor(out=sv[:, :, :], in0=sv[:, :, :], in1=cK[:, :].unsqueeze(1).broadcast_to([128, 64, 16]), op=ALU.add), reads=['sv', 'cK'], writes=['sv'])
                        S.op('dve', lambda: V.tensor_tensor(out=sv[:, :, :], in0=sv[:, :, :], in1=Mcat[:, :, :], op=ALU.mult), reads=['sv', 'Mcat'], writes=['sv'])
                        S.op('dve', lambda: V.reduce_sum(out=slotf[:, :], in_=sv[:, :, :].rearrange("p t (k e) -> p (t k) e", k=2), axis=mybir.AxisListType.X), reads=['sv'], writes=['slotf'])
                        S.op('dve', lambda: V.tensor_copy(out=sloti[:, :], in_=slotf[:, :]), reads=['slotf'], writes=['sloti'])
                        S.op('dve', lambda: V.memset(eb[:, :], 0.0), writes=['ebf'])
                        for e in range(NE):
                            S.op('dve', lambda: V.tensor_scalar(out=cmp[:, :], in0=thr40[:, :], scalar1=base[:, e + 1:e + 2], scalar2=None, op0=ALU.is_gt), reads=['thr', 'base', 'cmp'], writes=['cmp'])
                            S.op('dve', lambda: V.tensor_tensor(out=eb[:, :], in0=eb[:, :], in1=cmp[:, :], op=ALU.add), reads=['cmp', 'ebf'], writes=['ebf'])
                        S.op('dve', lambda: V.tensor_scalar(out=eb[:, :], in0=eb[:, :], scalar1=float(NE - 1), scalar2=None, op0=ALU.min), reads=['ebf'], writes=['ebf'])
                        S.op('dve', lambda: V.tensor_scalar(out=eb[:, :], in0=eb[:, :], scalar1=float(DFE), scalar2=None, op0=ALU.mult), reads=['ebf'], writes=['ebf'])
                        S.barrier()
                        for tg in range(64):
                            par = tg % 2
                            S.dma('sp', xn2[:, par, :], H2_d[tg * 128:(tg + 1) * 128, :], writes=[('xn2', par)])
                            for k in range(2):
                                S.idma(G_d[:, :], bass.IndirectOffsetOnAxis(ap=sloti[:, tg * 2 + k:tg * 2 + k + 1], axis=0), xn2[:, par, :], None,
                                       reads=[('xn2', par), 'sloti'], writes=['G'])
                        S.barrier()
                        def load_block(j):
                            for tt in range(4):
                                S.dma('sp', xn2[:, tt, :], G_d[j * 512 + tt * 128: j * 512 + (tt + 1) * 128, :], reads=['G'], writes=[('xn2', tt)])
                                pb = PS[tt % 2][:, :].bitcast(BF16)
                                for k in range(8):
                                    S.op('pe', lambda: T.transpose(pb[:, k * 128:(k + 1) * 128], xn2[:, tt, k * 128:(k + 1) * 128], identb[:, :]),
                                         reads=[('xn2', tt), 'identb'], writes=[psk(tt % 2)], signal=(k == 7))
                                S.op('act', lambda: A.copy(out=hT2[:, :, tt * 128:(tt + 1) * 128], in_=pb.rearrange("p (k t) -> p k t", k=8)), reads=[psk(tt % 2)], writes=[('hT2', tt)])

                        load_block(0)
                        for j in range(NBLK):
                            wp = j % 2
                            S.op('dve', lambda: V.tensor_scalar(out=widxf[:, wp, :], in0=rowb[:, :], scalar1=eb[:, j:j + 1], scalar2=None, op0=ALU.add), reads=['ebf', 'rowb', ('widxf', wp)], writes=[('widxf', wp)])
                            S.op('dve', lambda: V.tensor_copy(out=widxi[:, wp, :], in_=widxf[:, wp, :]), reads=[('widxf', wp)], writes=[('widxi', wp)])
                            ffn_expert(None, None, None, lambda tt: None, sink=(widxi[:, wp, :], ('widxi', wp)))
                            if j + 1 < NBLK:
                                load_block(j + 1)
                            S.dma('sp', Y_d[j * 512:(j + 1) * 512, :].rearrange("(t p) d -> p t d", p=128), xt2[:, :, :], reads=[('xt2', tt) for tt in range(4)], writes=['Y'])
                        S.barrier()
                        for tg in range(64):
                            par = tg % 2
                            b, r0 = tg // 32, (tg % 32) * 128
                            xk = ('xt2', par)
                            S.dma('sp', xt2[:, par, :], xs[b, r0:r0 + 128, :], writes=[xk])
                            for k in range(2):
                                S.idma(yg[:, par, k, :], None, Y_d[:, :], bass.IndirectOffsetOnAxis(ap=sloti[:, tg * 2 + k:tg * 2 + k + 1], axis=0),
                                       reads=['sloti', 'Y'], writes=[('yg', par, k)])
                                S.op('dve', lambda: V.scalar_tensor_tensor(out=xt2[:, par, :], in0=yg[:, par, k, :], scalar=Wts[:, tg, k:k + 1], in1=xt2[:, par, :], op0=ALU.mult, op1=ALU.add),
                                     reads=[('yg', par, k), 'Wts', xk], writes=[xk])
                            if not last:
                                S.dma('sp', xs[b, r0:r0 + 128, :], xt2[:, par, :], reads=[xk], writes=[('xs', b)])
                            else:
                                si = ss_ctr[0] % 8
                                ss_ctr[0] += 1
                                sc = ss[:, si:si + 1]
                                sk = ('ss', si)
                                S.op('act', lambda: A.activation(out=junk[:, :], in_=xt2[:, par, :], func=AF.Square, accum_out=sc), reads=[xk], writes=['junk', sk])
                                S.op('act', lambda: A.activation(out=sc, in_=sc, func=AF.Ln, bias=epsc[:, 0:1], scale=1.0 / D), reads=[sk], writes=[sk])
                                S.op('act', lambda: A.activation(out=sc, in_=sc, func=AF.Exp, scale=-0.5), reads=[sk], writes=[sk])
                                S.op('dve', lambda: V.scalar_tensor_tensor(out=yfin[:, par, :], in0=xt2[:, par, :], scalar=sc, in1=gbc[:, :], op0=ALU.mult, op1=ALU.mult),
                                     reads=[xk, sk, 'gbc'], writes=[('yfin', par)])
                                S.dma('sp', y_out[b, r0:r0 + 128, :], yfin[:, par, :], reads=[('yfin', par)], writes=['y'])

                def ffn_expert(wgv, wuv, wd_fn, gcol_fn, sink=None, mid_hook=None):
                    hk = [('hT2', tt) for tt in range(4)]
                    dense_w = sink is None
                    for fcn in range(nfc):
                        sl = wctr[0] % 3
                        wctr[0] += 1
                        if dense_w:
                            S.dma('pool', wgs[:, sl, :, :].rearrange("p k f -> p (k f)"), wgv(fcn), writes=[('wgs', sl)])
                            S.dma('pool', wus[:, sl, :, :].rearrange("p k f -> p (k f)"), wuv(fcn), writes=[('wus', sl)])
                        else:
                            ioff = bass.IndirectOffsetOnAxis(ap=sink[0][:, fcn:fcn + 1], axis=0)
                            S.idma(wgs[:, sl, :, :].rearrange("p k f -> p (k f)"), None, I['moe_wg'], ioff, reads=[sink[1]], writes=[('wgs', sl)])
                            S.idma(wus[:, sl, :, :].rearrange("p k f -> p (k f)"), None, I['moe_wu'], ioff, reads=[sink[1]], writes=[('wus', sl)])
                        bg, bu = 2 * (fcn % 2), 2 * (fcn % 2) + 1
                        mm_group(bg, [(wgs[:, sl, dc, :], hT2[:, dc, :]) for dc in range(8)], hk + [('wgs', sl)])
                        mm_group(bu, [(wus[:, sl, dc, :], hT2[:, dc, :]) for dc in range(8)], hk + [('wus', sl)])
                        S.op('act', lambda: A.activation(out=sg[:, fcn % 2, :], in_=PS[bg][:, :], func=AF.Silu), reads=[psk(bg)], writes=[('sg', fcn % 2)])
                        S.op('dve', lambda: V.tensor_tensor(out=actT[:, fcn, :], in0=PS[bu][:, :], in1=sg[:, fcn % 2, :], op=ALU.mult),
                             reads=[psk(bu), ('sg', fcn % 2)], writes=[('actT', fcn)])
                    if mid_hook is not None:
                        mid_hook()
                    for fcn in range(nfc):
                        sl = wctr[0] % 3
                        wctr[0] += 1
                        if dense_w:
                            S.dma('pool', wds[:, sl, :], wd_fn(fcn), writes=[('wds', sl)])
                        else:
                            S.idma(wds[:, sl, :], None, I['moe_wd'], bass.IndirectOffsetOnAxis(ap=sink[0][:, fcn:fcn + 1], axis=0), reads=[sink[1]], writes=[('wds', sl)])
                        for tt in range(4):
                            for half in range(2):
                                S.op('pe', lambda: T.matmul(PS[tt * 2 + half][:, :], actT[:, fcn, tt * 128:(tt + 1) * 128], wds[:, sl, half * 512:(half + 1) * 512],
                                                            start=(fcn == 0), stop=(fcn == nfc - 1)),
                                     reads=[('actT', fcn), ('wds', sl)], writes=[psk(tt * 2 + half)], signal=(fcn == nfc - 1 or (tt == 3 and half == 1)))
                    for tt in range(4):
                        for half in range(2):
                            xv = xt2[:, tt, half * 512:(half + 1) * 512]
                            gc = gcol_fn(tt)
                            if sink is not None:
                                S.op('act' if half else 'dve', lambda: (A.copy if half else V.tensor_copy)(out=xv, in_=PS[tt * 2 + half][:, :]), reads=[psk(tt * 2 + half)], writes=[('xt2', tt)])
                            elif gc is None:
                                S.op('dve', lambda: V.tensor_tensor(out=xv, in0=PS[tt * 2 + half][:, :], in1=xv, op=ALU.add), reads=[psk(tt * 2 + half), ('xt2', tt)], writes=[('xt2', tt)])
                            else:
                                S.op('dve', lambda: V.scalar_tensor_tensor(out=xv, in0=PS[tt * 2 + half][:, :], scalar=gc, in1=xv, op0=ALU.mult, op1=ALU.add),
                                     reads=[psk(tt * 2 + half), ('xt2', tt), 'gw'], writes=[('xt2', tt)])

                for b in range(NBC):
                    for n in range(8):
                        tok0 = n * 512
                        S.dma('sp', xt2[:, :, :], xs[b, tok0:tok0 + 512, :].rearrange("(t p) d -> p t d", p=128), reads=[('xs', b)], writes=[('xt2', tt) for tt in range(4)])
                        norm_T_batch([(xt2[:, tt, :], ('xt2', tt), xn2[:, tt, :], ('xn2', tt), hT2[:, :, tt * 128:(tt + 1) * 128], ('hT2', tt), tt % 2) for tt in range(4)], gx, 'gx')
                        hk = [('hT2', tt) for tt in range(4)]
                        for h in range(4):
                            mm_group(2 + h % 2, [(XQ[:, dc, h * 128:(h + 1) * 128], hT2[:, dc, :]) for dc in range(8)], hk + ['xw_q'])
                            S.op('act', lambda: A.copy(out=qx[:, h, :], in_=PS[2 + h % 2][:, :]), reads=[psk(2 + h % 2)], writes=[('qx', h)])
                        def xscores(h):
                            for j in range(2):
                                bank = (4, 2)[h % 2] + j
                                S.op('pe', lambda: T.matmul(PS[bank][:, :], KmT[:, h, b * 256 + j * 128: b * 256 + (j + 1) * 128], qx[:, h, :], start=True, stop=True),
                                     reads=['KmT', ('qx', h)], writes=[psk(bank)])

                        xscores(0)
                        for h in range(4):
                            hp = h % 2
                            if h + 1 < 4:
                                xscores(h + 1)
                            for j in range(2):
                                bank = (4, 2)[hp] + j
                                S.op('act', lambda: A.activation(out=PTx[:, hp, j, :], in_=PS[bank][:, :], func=AF.Exp, scale=128 ** -0.5), reads=[psk(bank)], writes=[('PTx', hp, j)])
                            rk = [('PTx', hp, 0), ('PTx', hp, 1), 'Vm', 'onesb']
                            mm_group(6, [(Vm[:, b * 2 + j, h * 128:(h + 1) * 128], PTx[:, hp, j, :]) for j in range(2)], rk)
                            mm_group(7, [(onesb[:, :], PTx[:, hp, j, :]) for j in range(2)], rk)
                            S.op('act', lambda: A.activation(out=rdn[:, hp, :], in_=PS[7][:, :], func=AF.Ln), reads=[psk(7)], writes=[('rdn', hp)])
                            S.op('act', lambda: A.activation(out=rdn[:, hp, :], in_=rdn[:, hp, :], func=AF.Exp, scale=-1.0), reads=[('rdn', hp)], writes=[('rdn', hp)])
                            S.op('dve', lambda: V.tensor_tensor(out=ox[:, h, :], in0=PS[6][:, :], in1=rdn[:, hp, :], op=ALU.mult), reads=[psk(6), ('rdn', hp)], writes=[('ox', h)])
                        for tt in range(4):
                            for half in range(2):
                                mm_group(half, [(ox[:, h, tt * 128:(tt + 1) * 128], XO[:, h, half * 512:(half + 1) * 512]) for h in range(4)], [('ox', h) for h in range(4)] + ['xw_o'])
                                xv = xt2[:, tt, half * 512:(half + 1) * 512]
                                S.op('dve', lambda: V.tensor_tensor(out=xv, in0=PS[half][:, :], in1=xv, op=ALU.add), reads=[psk(half), ('xt2', tt)], writes=[('xt2', tt)])
                        if not moe:
                            norm_T_batch([(xt2[:, tt, :], ('xt2', tt), xn2[:, tt, :], ('xn2', tt), hT2[:, :, tt * 128:(tt + 1) * 128], ('hT2', tt), tt % 2) for tt in range(4)], gf, 'gf')
                            ffn_expert(lambda fcn: I['ffn_wg'][fcn * 128:(fcn + 1) * 128, :], lambda fcn: I['ffn_wu'][fcn * 128:(fcn + 1) * 128, :], lambda fcn: I['ffn_wd'][fcn * 128:(fcn + 1) * 128, :], lambda tt: None)
                        else:
                            for tt in range(4):
                                si = ss_ctr[0] % 8
                                ss_ctr[0] += 1
                                sc = ss[:, si:si + 1]
                                sk = ('ss', si)
                                xk = ('xt2', tt)
                                S.op('act', lambda: A.activation(out=junk[:, :], in_=xt2[:, tt, :], func=AF.Square, accum_out=sc), reads=[xk], writes=['junk', sk])
                                S.op('act', lambda: A.activation(out=sc, in_=sc, func=AF.Ln, bias=epsc[:, 0:1], scale=1.0 / D), reads=[sk], writes=[sk])
                                S.op('act', lambda: A.activation(out=sc, in_=sc, func=AF.Exp, scale=-0.5), reads=[sk], writes=[sk])
                                S.op('dve', lambda: V.scalar_tensor_tensor(out=hn32[:, :], in0=xt2[:, tt, :], scalar=sc, in1=gf[:, :], op0=ALU.mult, op1=ALU.mult),
                                     reads=[xk, sk, 'gf'], writes=['hn32'])
                                for k in range(8):
                                    S.op('pe', lambda: T.transpose(PS[k // 4][:, (k % 4) * 128:(k % 4 + 1) * 128], hn32[:, k * 128:(k + 1) * 128], identf[:, :]),
                                         reads=['hn32', 'identf'], writes=[psk(k // 4)], signal=(k % 4 == 3))
                                for hb in range(2):
                                    S.op('act', lambda: A.copy(out=hT32[:, hb * 4:(hb + 1) * 4, :], in_=PS[hb][:, :].rearrange("p (k t) -> p k t", k=4)), reads=[psk(hb)], writes=['hT32'])
                                for dc in range(8):
                                    S.op('pe', lambda: T.matmul(PS[2][:, 0:NE], hT32[:, dc, :], wr[:, dc, :], start=(dc == 0), stop=(dc == 7)),
                                         reads=['hT32', 'wr'], writes=[psk(2)], signal=(dc == 7))
                                S.op('dve', lambda: V.tensor_copy(out=lg[:, tt, :], in_=PS[2][:, 0:NE]), reads=[psk(2)], writes=['lg'])
                                S.op('dve', lambda: V.max(out=m8[:, tt, :], in_=lg[:, tt, :]), reads=['lg'], writes=['m8'])
                                tile_g = (b * 8 + n) * 4 + tt
                                S.op('dve', lambda: V.tensor_scalar(out=Mcat[:, tile_g, 0:8], in0=lg[:, tt, :], scalar1=m8[:, tt, 0:1], scalar2=None, op0=ALU.is_equal), reads=['lg', 'm8'], writes=['Mcat'])
                                S.op('dve', lambda: V.tensor_scalar(out=Mcat[:, tile_g, 8:16], in0=lg[:, tt, :], scalar1=m8[:, tt, 1:2], scalar2=None, op0=ALU.is_equal), reads=['lg', 'm8'], writes=['Mcat'])
                                S.op('dve', lambda: V.tensor_tensor(out=m8[:, tt, 2:3], in0=m8[:, tt, 1:2], in1=m8[:, tt, 0:1], op=ALU.subtract), reads=['m8'], writes=['m8'])
                                S.op('act', lambda: A.activation(out=m8[:, tt, 2:3], in_=m8[:, tt, 2:3], func=AF.Exp), reads=['m8'], writes=['m8'])
                                S.op('dve', lambda: V.tensor_scalar(out=m8[:, tt, 2:3], in0=m8[:, tt, 2:3], scalar1=1.0, scalar2=None, op0=ALU.add), reads=['m8'], writes=['m8'])
                                S.op('dve', lambda: V.reciprocal(out=Wts[:, tile_g, 0:1], in_=m8[:, tt, 2:3]), reads=['m8'], writes=['Wts'])
                                S.op('dve', lambda: V.tensor_scalar(out=Wts[:, tile_g, 1:2], in0=Wts[:, tile_g, 0:1], scalar1=-1.0, scalar2=1.0, op0=ALU.mult, op1=ALU.add), reads=['Wts'], writes=['Wts'])
                                S.op('act', lambda: A.copy(out=xn2[:, tt % 2, :], in_=hn32[:, :]), reads=['hn32'], writes=[('xn2', tt % 2)])
                                r0 = b * L + tok0 + tt * 128
                                S.dma('sp', H2_d[r0:r0 + 128, :], xn2[:, tt % 2, :], reads=[('xn2', tt % 2)], writes=['H2'])
                        if moe or not last:
                            S.dma('sp', xs[b, tok0:tok0 + 512, :].rearrange("(t p) d -> p t d", p=128), xt2[:, :, :], reads=[('xt2', tt) for tt in range(4)], writes=[('xs', b)])
                        if last and not moe:
                            for tt in range(4):
                                si = ss_ctr[0] % 8
                                ss_ctr[0] += 1
                                sc = ss[:, si:si + 1]
                                sk = ('ss', si)
                                xk = ('xt2', tt)
                                S.op('act', lambda: A.activation(out=junk[:, :], in_=xt2[:, tt, :], func=AF.Square, accum_out=sc), reads=[xk], writes=['junk', sk])
                                S.op('act', lambda: A.activation(out=sc, in_=sc, func=AF.Ln, bias=epsc[:, 0:1], scale=1.0 / D), reads=[sk], writes=[sk])
                                S.op('act', lambda: A.activation(out=sc, in_=sc, func=AF.Exp, scale=-0.5), reads=[sk], writes=[sk])
                                S.op('dve', lambda: V.scalar_tensor_tensor(out=yfin[:, tt % 2, :], in0=xt2[:, tt, :], scalar=sc, in1=gbc[:, :], op0=ALU.mult, op1=ALU.mult),
                                     reads=[xk, sk, 'gbc'], writes=[('yfin', tt % 2)])
                                S.dma('sp', y_out[b, tok0 + tt * 128: tok0 + (tt + 1) * 128, :], yfin[:, tt % 2, :], reads=[('yfin', tt % 2)], writes=['y'])
                if moe:
                    moe_sparse(last)
                S.barrier()
        with ExitStack() as es_:
            junk = es_.enter_context(nc.sbuf_tensor(UN("junk"), [128, D], BF16))
            S.dma('sp', identb[:, :], I['identb'], writes=['identb'])
            S.dma('sp', mprev[:, :], I['mprev'], writes=['mprev'])
            S.dma('sp', mnext[:, :], I['mnext'], writes=['mnext'])
            S.op('dve', lambda: V.memset(onesb[:, :], 1.0), writes=['onesb'])
            S.op('dve', lambda: V.memset(onesm[:, :, :], 0.0), writes=['onesm'])
            S.op('dve', lambda: V.memset(onesm[:, 0, 0:64], 1.0), reads=['onesm'], writes=['onesm'])
            S.op('dve', lambda: V.memset(onesm[:, 1, 64:128], 1.0), reads=['onesm'], writes=['onesm'])
            S.op('dve', lambda: V.memset(epsc[:, :], EPS), writes=['epsc'])
            S.op('dve', lambda: V.memset(npi[:, :], -PI), writes=['npi'])

            phase_mem()
            for l in range(n_layers):
                phase_F(l)
                phase_AB(l)
                phase_H(l)
                phase_O(l)
                phase_XF(l)
            S.barrier()
    return nc


_PROG = {}


def _slab_layout(w):
    E, Dd, F = w.shape
    return np.ascontiguousarray(w.reshape(E, Dd // 128, 128, F // 128, 128).transpose(0, 3, 2, 1, 4)).reshape(E * F, Dd)


def _prep_inputs(inp):
    c = host_constants()
    f32 = lambda a: np.ascontiguousarray(np.asarray(a, dtype=np.float32))
    perm = attn_row_perm()
    w_out = f32(inp['w_out']).copy()
    w_out[:, :512, :] = w_out[:, perm, :]
    shared = {
        'mem_norm': f32(inp['mem_norm']), 'mix_norm': f32(inp['mix_norm']),
        'wa': np.stack([permute_w_in(f32(inp['w_in'])[l]) for l in range(2)]),
        'attn_sink': f32(inp['attn_sink']), 'cw': f32(inp['hy_conv_w']), 'cb': f32(inp['hy_conv_b']),
        'f_w1': f32(inp['hy_f_w1']), 'f_b1': f32(inp['hy_f_b1']), 'f_fr1': f32(inp['hy_f_freq1']),
        'f_w2': f32(inp['hy_f_w2']), 'f_b2': f32(inp['hy_f_b2']), 'f_fr2': f32(inp['hy_f_freq2']),
        'f_w3': f32(inp['hy_f_w3']), 'skip': f32(inp['hy_skip']),
        'ga': np.ascontiguousarray(f32(inp['attn_out_norm'])[:, perm]), 'gy': f32(inp['hy_out_norm']),
        'w_out': w_out, 'xattn_norm': f32(inp['xattn_norm']),
        'xw_q': f32(inp['xw_q']), 'xw_k': f32(inp['xw_k']), 'xw_v': f32(inp['xw_v']), 'xw_o': f32(inp['xw_o']),
        'ffn_norm': f32(inp['ffn_norm']), 'ffn_wg': _slab_layout(f32(inp['ffn_w_gate'])), 'ffn_wu': _slab_layout(f32(inp['ffn_w_up'])),
        'ffn_wd': f32(inp['ffn_w_down'])[0], 'moe_router': f32(inp['moe_router'])[0],
        'moe_wg': _slab_layout(f32(inp['moe_w_gate'])[0]), 'moe_wu': _slab_layout(f32(inp['moe_w_up'])[0]), 'moe_wd': f32(inp['moe_w_down'])[0].reshape(NE * DFE, D),
        'final_norm': f32(inp['final_norm']),
    }
    for k in ('identb', 'mprev', 'mnext', 'ropec', 'ropes', 'dftc', 'dfts', 'dftnq', 'dfcf', 'dfsf', 'dfci', 'dfsi', 'wcol', 'zT', 'decay', 'decay_rev', 'ltb', 'slt65', 'thr32', 'thr40', 'rowbase'):
        shared[k] = c[k]
    return shared


def kernel(**inputs):
    x = np.asarray(inputs['x'], dtype=np.float32)
    mem = np.asarray(inputs['mem'], dtype=np.float32)
    shared = _prep_inputs(inputs)
    if 'nc' not in _PROG:
        _PROG['nc'] = build_program()
    nc = _PROG['nc']
    in_maps = []
    for c in range(NCORES):
        m = dict(shared)
        m['x'] = np.ascontiguousarray(x[c * NBC:(c + 1) * NBC])
        m['mem'] = np.ascontiguousarray(mem[c * NBC:(c + 1) * NBC].reshape(NBC * MEM, D))
        in_maps.append(m)
    res = run_bass_kernel_spmd(nc, in_maps, core_ids=list(range(NCORES)))
    return np.concatenate([np.asarray(r['y']) for r in res.results], axis=0).astype(np.float32)
```

```python
import math
from contextlib import ExitStack
import numpy as np
import ml_dtypes
import concourse.bass as bass
import concourse.mybir as mybir
from concourse.bass_utils import run_bass_kernel_spmd

F32 = mybir.dt.float32
BF16 = mybir.dt.bfloat16
AF = mybir.ActivationFunctionType
ALU = mybir.AluOpType
BF = ml_dtypes.bfloat16

D = 1024
L = 4096
NBC = 2
NCORES = 8
MEM = 256
DFF = 2816
NE = 8
DFE = 3584
EPS = 1e-6
NBLK = NBC * L * 2 // 512 + NE
NWA = 23
PI = math.pi


class Sched:
    NDS = 8

    def __init__(self, nc):
        self.nc = nc
        self.eng = {'pe': nc.tensor, 'act': nc.scalar, 'dve': nc.vector, 'pool': nc.gpsimd, 'sp': nc.sync}
        self.sem = {e: nc.alloc_semaphore(f"s_{e}") for e in ('pe', 'act', 'dve', 'pool')}
        self.cnt = {e: 0 for e in self.sem}
        self.dsem = {q: [nc.alloc_semaphore(f"d_{q}{i}") for i in range(self.NDS)] for q in ('sp', 'pool', 'act')}
        self.dcnt = {(q, i): 0 for q in self.dsem for i in range(self.NDS)}
        self.dnext = {q: 0 for q in self.dsem}
        self.waited = {}
        self.last_w = {}
        self.reads = {}
        self.pe_pending = ([], [])

    def _wait(self, e, tok):
        if tok[0] == 'c':
            _, e2, c = tok
            if e2 == e and e == 'pe':
                return
            key = (e, 'c', e2)
            if self.waited.get(key, 0) >= c:
                return
            self.waited[key] = c
            self.eng[e].wait_ge(self.sem[e2], c)
        else:
            _, q, i, c = tok
            key = (e, 'd', q, i)
            if self.waited.get(key, 0) >= c:
                return
            self.waited[key] = c
            self.eng[e].wait_ge(self.dsem[q][i], 16 * c)

    def _deps(self, e, reads, writes):
        toks = []
        for r in reads:
            if r in self.last_w:
                toks.append(self.last_w[r])
        for w in writes:
            if w in self.last_w:
                toks.append(self.last_w[w])
            toks.extend(self.reads.get(w, []))
        for t in toks:
            self._wait(e, t)

    def _record(self, tok, reads, writes):
        for w in writes:
            self.last_w[w] = tok
            self.reads[w] = []
        for r in reads:
            if r not in writes:
                self.reads.setdefault(r, []).append(tok)

    def op(self, e, fn, reads=(), writes=(), signal=True):
        reads = list(reads)
        writes = list(writes)
        self._deps(e, reads, writes)
        ins = fn()
        if e == 'pe' and not signal:
            self.pe_pending[0].extend(reads)
            self.pe_pending[1].extend(writes)
            return ins
        ins.then_inc(self.sem[e], 1)
        self.cnt[e] += 1
        tok = ('c', e, self.cnt[e])
        if e == 'pe':
            reads += self.pe_pending[0]
            writes += self.pe_pending[1]
            self.pe_pending = ([], [])
        self._record(tok, reads, writes)
        return ins

    def dma(self, q, out, in_, reads=(), writes=(), **kw):
        i = self.dnext[q]
        self.dnext[q] = (i + 1) % self.NDS
        c = self.dcnt[(q, i)]
        if c > 0:
            self._wait(q, ('d', q, i, c))
        self._deps(q, reads, writes)
        ins = self.eng[q].dma_start(out=out, in_=in_, **kw)
        ins.then_inc(self.dsem[q][i], 16)
        self.dcnt[(q, i)] = c + 1
        tok = ('d', q, i, c + 1)
        self._record(tok, list(reads), list(writes))
        return ins

    def idma(self, out, out_offset, in_, in_offset, reads=(), writes=()):
        q = 'pool'
        i = self.dnext[q]
        self.dnext[q] = (i + 1) % self.NDS
        c = self.dcnt[(q, i)]
        if c > 0:
            self._wait(q, ('d', q, i, c))
        self._deps(q, reads, writes)
        ins = self.nc.gpsimd.indirect_dma_start(out=out, out_offset=out_offset, in_=in_, in_offset=in_offset)
        ins.then_inc(self.dsem[q][i], 16)
        self.dcnt[(q, i)] = c + 1
        tok = ('d', q, i, c + 1)
        self._record(tok, list(reads), list(writes))
        return ins

    def barrier(self):
        assert not self.pe_pending[0] and not self.pe_pending[1]
        for e in self.eng:
            for e2 in self.sem:
                if e2 != e and self.cnt[e2] > 0:
                    self._wait(e, ('c', e2, self.cnt[e2]))
            for (q, i), c in self.dcnt.items():
                if c > 0:
                    self._wait(e, ('d', q, i, c))
        self.last_w = {}
        self.reads = {}


_CONST = {}


def host_constants():
    if _CONST:
        return _CONST
    c = {}
    c['identb'] = np.eye(128, dtype=np.float32).astype(BF)
    kt = np.arange(128)[:, None]
    qt = np.arange(128)[None, :]
    c['mprev'] = (kt >= qt).astype(np.float32).astype(BF)
    c['mnext'] = (kt <= qt).astype(np.float32).astype(BF)
    pos = np.arange(L, dtype=np.float32)
    inv = (10000.0 ** (-np.arange(0, 64, 2, dtype=np.float32) / 64)).astype(np.float32)
    ang = pos[:, None] * inv[None, :]
    cos, sin = np.cos(ang).astype(np.float32), np.sin(ang).astype(np.float32)
    dh = np.arange(128) % 64
    c['ropec'] = np.ascontiguousarray(cos[:, dh % 32].T).astype(BF)
    sgn = np.where(dh < 32, -1.0, 1.0).astype(np.float32)
    c['ropes'] = np.ascontiguousarray((sin[:, dh % 32] * sgn[None, :]).T).astype(BF)
    N = 2 * L
    fperm = np.concatenate([np.arange(0, L, 2), np.arange(1, L, 2)]).astype(np.int64)
    idx = (np.arange(L, dtype=np.int64)[:, None] * fperm[None, :]) % N
    ang = idx.astype(np.float64) * (2 * np.pi / N)
    c['dftc'] = np.cos(ang).astype(np.float32).astype(BF)
    c['dfts'] = np.sin(ang).astype(np.float32).astype(BF)
    alt = np.where(np.arange(L) % 2 == 0, 1.0, -1.0)
    idx2 = ((2 * np.arange(L // 2, dtype=np.int64)[:, None] + 1) * fperm[None, :]) % (2 * N)
    ang2 = idx2.astype(np.float64) * (2 * np.pi / (2 * N))
    cf = np.cos(ang2)
    sf = np.sin(ang2)
    sf[:, 0] = alt[:L // 2]
    c['dfcf'] = cf.astype(np.float32).astype(BF)
    c['dfsf'] = sf.astype(np.float32).astype(BF)
    c['dfci'] = np.ascontiguousarray(cf.T).astype(np.float32).astype(BF)
    c['dfsi'] = np.ascontiguousarray(sf.T).astype(np.float32).astype(BF)
    nq = np.zeros((L, 128), np.float32)
    nq[:, 0] = alt
    c['dftnq'] = nq.astype(BF)
    w = np.full((L,), 2.0 / N, np.float32)
    w[0] = 1.0 / N
    c['wcol'] = np.ascontiguousarray(w.reshape(32, 128).T)
    t = np.linspace(0.0, 1.0, L, dtype=np.float32)[:, None]
    wv = (2.0 * np.float32(math.pi) * np.arange(L, dtype=np.float32)[:, None] / np.float32(L)).astype(np.float32)
    f = np.linspace(1e-4, 15.0, 16, dtype=np.float32)[None, :]
    z = np.concatenate([t, np.cos(f * wv), -np.sin(f * wv)], axis=-1).astype(np.float32)
    c['zT'] = np.ascontiguousarray(z.T)
    mn = math.log(1e-2) / 1.5
    mx = math.log(1e-2) / 0.3
    deltas = np.linspace(mn, mx, 1024, dtype=np.float32)
    dec = np.exp(-t * np.abs(deltas)[None, :]).astype(np.float32)
    dec4 = np.concatenate([dec, dec], axis=1)
    dec4[0, 1024:] = 0.0
    c['decay'] = np.ascontiguousarray(dec4)
    drev = np.zeros((L // 2, 2048), np.float32)
    drev[1:] = dec4[L - 1:L // 2:-1]
    c['decay_rev'] = drev
    k_ = np.arange(128)[:, None]
    m_ = np.arange(128)[None, :]
    c['ltb'] = (k_ < m_).astype(np.float32).astype(BF)
    slt = (np.arange(64)[:, None] < np.arange(65)[None, :]).astype(np.float32)
    slt[:, 64] = 1.0
    c['slt65'] = slt.astype(BF)
    c['rowbase'] = (128.0 * np.arange(DFE // 128, dtype=np.float32)[None, :] + np.arange(128, dtype=np.float32)[:, None]).astype(np.float32)
    c['thr32'] = np.tile((512.0 * np.arange(32, dtype=np.float32))[None, :], (128, 1))
    c['thr40'] = np.tile((512.0 * np.arange(NBLK, dtype=np.float32) + 0.5)[None, :], (128, 1))
    _CONST.update(c)
    return c


def permute_w_in(w_in_l):
    cols = []
    for swap in (0, 1):
        for g in range(4):
            for kvh in range(2):
                h = kvh * 4 + g
                dh = (np.arange(64) + 32 * swap) % 64
                cols.append(h * 64 + dh)
    q = np.concatenate(cols)
    kc = []
    for swap in (0, 1):
        for kvh in range(2):
            dh = (np.arange(64) + 32 * swap) % 64
            kc.append(512 + kvh * 64 + dh)
    order = np.concatenate([q, np.concatenate(kc), np.arange(640, 768), np.arange(768, 2304)])
    return np.ascontiguousarray(w_in_l[:, order])


def attn_row_perm():
    rows = []
    for g in range(4):
        for kvh in range(2):
            rows.append((kvh * 4 + g) * 64 + np.arange(64))
    return np.concatenate(rows)


def build_program(n_layers=2, taps=(), debug=False):
    nc = bass.Bass("TRN2", target_bir_lowering=False)
    S = Sched(nc)
    I = {}

    def din(name, shape, dt=F32):
        I[name] = nc.dram_tensor(name, list(shape), dt, kind="ExternalInput").ap()
        return I[name]

    def dscr(name, shape, dt):
        return nc.dram_tensor(name, list(shape), dt, kind=("ExternalOutput" if debug else "Internal")).ap()

    x_in = din('x', [NBC, L, D])
    mem_in = din('mem', [NBC * MEM, D])
    din('mem_norm', [D]); din('mix_norm', [2, D]); din('wa', [2, D, NWA * 128]); din('attn_sink', [2, 8])
    din('cw', [2, 3, 1536]); din('cb', [2, 1536])
    din('f_w1', [2, 33, 64]); din('f_b1', [2, 64]); din('f_fr1', [2, 64]); din('f_w2', [2, 64, 64])
    din('f_b2', [2, 64]); din('f_fr2', [2, 64]); din('f_w3', [2, 64, 2048]); din('skip', [2, 2, 512])
    din('ga', [2, 512]); din('gy', [2, 512]); din('w_out', [2, D, D]); din('xattn_norm', [2, D])
    din('xw_q', [2, D, 512]); din('xw_k', [2, D, 512]); din('xw_v', [2, D, 512]); din('xw_o', [2, 512, D])
    din('ffn_norm', [2, D]); din('ffn_wg', [DFF, D]); din('ffn_wu', [DFF, D]); din('ffn_wd', [DFF, D])
    din('moe_router', [D, NE]); din('moe_wg', [NE * DFE, D]); din('moe_wu', [NE * DFE, D]); din('moe_wd', [NE * DFE, D])
    din('final_norm', [D])
    din('identb', [128, 128], BF16); din('mprev', [128, 128], BF16); din('mnext', [128, 128], BF16)
    din('ropec', [128, L], BF16); din('ropes', [128, L], BF16)
    din('dftc', [L, L], BF16); din('dfts', [L, L], BF16); din('dftnq', [L, 128], BF16)
    din('dfcf', [L // 2, L], BF16); din('dfsf', [L // 2, L], BF16); din('dfci', [L, L // 2], BF16); din('dfsi', [L, L // 2], BF16)
    din('wcol', [128, 32]); din('zT', [33, L]); din('decay', [L, 2048]); din('decay_rev', [L // 2, 2048])
    din('ltb', [128, 128], BF16); din('slt65', [64, 65], BF16); din('thr32', [128, 32]); din('thr40', [128, NBLK]); din('rowbase', [128, DFE // 128])
    y_out = nc.dram_tensor('y', [NBC, L, D], F32, kind="ExternalOutput").ap()

    xs = dscr('xs', [NBC, L, D], F32)
    U_d = dscr('U_d', [NBC, 12, 128, L + 8], BF16)
    ZZ_d = dscr('ZZ_d', [4, 128, L], BF16)
    aT_d = dscr('aT_d', [NBC, 128, 4, L], BF16)
    yT_d = dscr('yT_d', [NBC, 128, 4, L], BF16)
    AB_d = dscr('AB_d', [2, 2, L, 512], BF16)
    H2_d = dscr('H2_d', [NBC * L, D], BF16)
    G_d = dscr('G_d', [NBLK * 512, D], BF16)
    Y_d = dscr('Y_d', [NBLK * 512, D], F32)
    tapd = {}
    for nm, shape, dt in taps:
        tapd[nm] = nc.dram_tensor('tap_' + nm, list(shape), dt, kind="ExternalOutput").ap()

    V, A, P, T = nc.vector, nc.scalar, nc.gpsimd, nc.tensor
    uctr = [0]

    def UN(n):
        uctr[0] += 1
        return f"{n}_{uctr[0]}"

    with ExitStack() as es_:
        identb = es_.enter_context(nc.sbuf_tensor(UN("identb"), [128, 128], BF16))
        mprev = es_.enter_context(nc.sbuf_tensor(UN("mprev"), [128, 128], BF16))
        mnext = es_.enter_context(nc.sbuf_tensor(UN("mnext"), [128, 128], BF16))
        onesb = es_.enter_context(nc.sbuf_tensor(UN("onesb"), [128, 128], BF16))
        onesm = es_.enter_context(nc.sbuf_tensor(UN("onesm"), [128, 2, 128], BF16))
        epsc = es_.enter_context(nc.sbuf_tensor(UN("epsc"), [128, 1], F32))
        npi = es_.enter_context(nc.sbuf_tensor(UN("npi"), [128, 1], F32))
        mT = es_.enter_context(nc.sbuf_tensor(UN("mT"), [128, 8, NBC * MEM], BF16))
        KmT = es_.enter_context(nc.sbuf_tensor(UN("KmT"), [128, 4, NBC * MEM], BF16))
        Vm = es_.enter_context(nc.sbuf_tensor(UN("Vm"), [128, NBC * 2, 512], BF16))
        ss = es_.enter_context(nc.sbuf_tensor(UN("ss"), [128, 8], F32))
        gbc = es_.enter_context(nc.sbuf_tensor(UN("gbc"), [128, D], F32))
        ps0 = es_.enter_context(nc.psum_tensor(UN("ps0"), [128, 512], F32))
        ps1 = es_.enter_context(nc.psum_tensor(UN("ps1"), [128, 512], F32))
        ps2 = es_.enter_context(nc.psum_tensor(UN("ps2"), [128, 512], F32))
        ps3 = es_.enter_context(nc.psum_tensor(UN("ps3"), [128, 512], F32))
        ps4 = es_.enter_context(nc.psum_tensor(UN("ps4"), [128, 512], F32))
        ps5 = es_.enter_context(nc.psum_tensor(UN("ps5"), [128, 512], F32))
        ps6 = es_.enter_context(nc.psum_tensor(UN("ps6"), [128, 512], F32))
        ps7 = es_.enter_context(nc.psum_tensor(UN("ps7"), [128, 512], F32))
        PS = [ps0, ps1, ps2, ps3, ps4, ps5, ps6, ps7]

        def psk(i):
            return ('ps', i)

        def load_gain(vec_ap):
            with nc.allow_non_contiguous_dma(reason="bcast"):
                S.dma('sp', gbc[:, :], vec_ap.partition_broadcast(128), writes=['gbc'])

        ss_ctr = [0]

        def norm_T(x_ap, xn_ap, xnk, hT_ap, hTk, bank, xk, gt=None, gk='gbc'):
            si = ss_ctr[0] % 8
            ss_ctr[0] += 1
            sc = ss[:, si:si + 1]
            sk = ('ss', si)
            gt = gbc if gt is None else gt
            S.op('act', lambda: A.activation(out=junk[:, :], in_=x_ap, func=AF.Square, accum_out=sc),
                 reads=[xk], writes=['junk', sk])
            S.op('act', lambda: A.activation(out=sc, in_=sc, func=AF.Ln, bias=epsc[:, 0:1], scale=1.0 / D), reads=[sk], writes=[sk])
            S.op('act', lambda: A.activation(out=sc, in_=sc, func=AF.Exp, scale=-0.5), reads=[sk], writes=[sk])
            S.op('dve', lambda: V.scalar_tensor_tensor(out=xn_ap, in0=x_ap, scalar=sc, in1=gt[:, :], op0=ALU.mult, op1=ALU.mult),
                 reads=[xk, sk, gk], writes=[xnk])
            pb = PS[bank][:, :].bitcast(BF16)
            for k in range(8):
                S.op('pe', lambda: T.transpose(pb[:, k * 128:(k + 1) * 128], xn_ap[:, k * 128:(k + 1) * 128], identb[:, :]),
                     reads=[xnk, 'identb'], writes=[psk(bank)], signal=(k == 7))
            S.op('act', lambda: A.copy(out=hT_ap, in_=pb.rearrange("p (k t) -> p k t", k=8)), reads=[psk(bank)], writes=[hTk])

        def norm_T_batch(items, gt=None, gk='gbc'):
            gt = gbc if gt is None else gt
            scs = []
            for (x_ap, xk, xn_ap, xnk, hT_ap, hTk, bank) in items:
                si = ss_ctr[0] % 8
                ss_ctr[0] += 1
                sc, sk = ss[:, si:si + 1], ('ss', si)
                scs.append((sc, sk))
                S.op('act', lambda: A.activation(out=junk[:, :], in_=x_ap, func=AF.Square, accum_out=sc), reads=[xk], writes=['junk', sk])
                S.op('act', lambda: A.activation(out=sc, in_=sc, func=AF.Ln, bias=epsc[:, 0:1], scale=1.0 / D), reads=[sk], writes=[sk])
                S.op('act', lambda: A.activation(out=sc, in_=sc, func=AF.Exp, scale=-0.5), reads=[sk], writes=[sk])
            for (x_ap, xk, xn_ap, xnk, hT_ap, hTk, bank), (sc, sk) in zip(items, scs):
                S.op('dve', lambda: V.scalar_tensor_tensor(out=xn_ap, in0=x_ap, scalar=sc, in1=gt[:, :], op0=ALU.mult, op1=ALU.mult),
                     reads=[xk, sk, gk], writes=[xnk])
            for (x_ap, xk, xn_ap, xnk, hT_ap, hTk, bank) in items:
                pb = PS[bank][:, :].bitcast(BF16)
                for k in range(8):
                    S.op('pe', lambda: T.transpose(pb[:, k * 128:(k + 1) * 128], xn_ap[:, k * 128:(k + 1) * 128], identb[:, :]),
                         reads=[xnk, 'identb'], writes=[psk(bank)], signal=(k == 7))
                S.op('act', lambda: A.copy(out=hT_ap, in_=pb.rearrange("p (k t) -> p k t", k=8)), reads=[psk(bank)], writes=[hTk])

        def mm_group(bank, pairs, rk, col0=0, ncol=512):
            n = len(pairs)
            for i, (lt, rh) in enumerate(pairs):
                S.op('pe', lambda: T.matmul(PS[bank][:, col0:col0 + ncol], lt, rh, start=(i == 0), stop=(i == n - 1)),
                     reads=rk, writes=[psk(bank)], signal=(i == n - 1))

        def phase_mem():
            with ExitStack() as es_:
                mx = es_.enter_context(nc.sbuf_tensor(UN("mx"), [128, 4, D], F32))
                mxn = es_.enter_context(nc.sbuf_tensor(UN("mxn"), [128, D], BF16))
                load_gain(I['mem_norm'])
                S.dma('sp', mx[:, :, :], mem_in.rearrange("(n p) d -> p n d", p=128), writes=['mx'])
                for n in range(4):
                    norm_T(mx[:, n, :], mxn[:, :], 'mxn', mT[:, :, n * 128:(n + 1) * 128], ('mT', n), n % 2, 'mx')
                S.barrier()

        def phase_F(l):
            with ExitStack() as es_:
                zTs = es_.enter_context(nc.sbuf_tensor(UN("zTs"), [33, 2, 512], F32))
                fw1 = es_.enter_context(nc.sbuf_tensor(UN("fw1"), [33, 64], F32))
                fw2 = es_.enter_context(nc.sbuf_tensor(UN("fw2"), [64, 64], F32))
                fw3 = es_.enter_context(nc.sbuf_tensor(UN("fw3"), [64, 2048], F32))
                fcol = es_.enter_context(nc.sbuf_tensor(UN("fcol"), [64, 8], F32))
                h1T = es_.enter_context(nc.sbuf_tensor(UN("h1T"), [64, L], F32))
                h2T = es_.enter_context(nc.sbuf_tensor(UN("h2T"), [64, L], F32))
                ftmp = es_.enter_context(nc.sbuf_tensor(UN("ftmp"), [64, 3, 512], F32))
                eb = es_.enter_context(nc.sbuf_tensor(UN("eb"), [128, 32, 512], BF16))
                ob = es_.enter_context(nc.sbuf_tensor(UN("ob"), [128, 32, 512], BF16))
                dct = es_.enter_context(nc.sbuf_tensor(UN("dct"), [128, 2, 2, 512], F32))
                kft = es_.enter_context(nc.sbuf_tensor(UN("kft"), [128, 2, 2, 512], F32))
                skr = es_.enter_context(nc.sbuf_tensor(UN("skr"), [1, 2, 512], F32))
                slab = es_.enter_context(nc.sbuf_tensor(UN("slab"), [128, 4, 2, 512], BF16))
                nqs = es_.enter_context(nc.sbuf_tensor(UN("nqs"), [128, 16, 128], BF16))
                d2048 = es_.enter_context(nc.sbuf_tensor(UN("d2048"), [1, 2, 512], F32))
                k2048 = es_.enter_context(nc.sbuf_tensor(UN("k2048"), [1, 2, 512], F32))
                eo2048 = es_.enter_context(nc.sbuf_tensor(UN("eo2048"), [1, 2, 512], BF16))
                rows = es_.enter_context(nc.sbuf_tensor(UN("rows"), [1, 2, 2, 512], BF16))
                wcol = es_.enter_context(nc.sbuf_tensor(UN("wcol"), [128, 32], F32))
                abo = es_.enter_context(nc.sbuf_tensor(UN("abo"), [128, 2, 2, 512], BF16))
                S.dma('sp', fw1[:, :], I['f_w1'][l], writes=['fw'])
                S.dma('sp', fw2[:, :], I['f_w2'][l], writes=['fw'])
                S.dma('sp', fw3[:, :], I['f_w3'][l], writes=['fw'])
                S.dma('sp', wcol[:, :], I['wcol'], writes=['wcol'])
                S.dma('sp', nqs[:, :, :], I['dftnq'][0:L // 2, :].rearrange("(k p) f -> p k f", p=128), writes=['nqs'])
                S.dma('sp', skr[:, :, :], I['skip'][l].unsqueeze(0), writes=['skr'])
                with nc.allow_non_contiguous_dma(reason="tiny"):
                    for ci, nm in enumerate(('f_b1', 'f_fr1', 'f_b2', 'f_fr2')):
                        S.dma('sp', fcol[:, ci:ci + 1], I[nm][l].unsqueeze(1), writes=['fcol'])
                S.op('dve', lambda: V.tensor_tensor(out=fcol[:, 4:5], in0=fcol[:, 0:1], in1=fcol[:, 1:2], op=ALU.mult), reads=['fcol'], writes=['fcol'])
                S.op('dve', lambda: V.tensor_tensor(out=fcol[:, 5:6], in0=fcol[:, 2:3], in1=fcol[:, 3:4], op=ALU.mult), reads=['fcol'], writes=['fcol'])

                def sin_layer(w_ap, src, dst, frc, fbc, kdim):
                    for n in range(8):
                        bank = n % 2
                        if src is None:
                            S.dma('sp', zTs[:, n % 2, :], I['zT'][:, n * 512:(n + 1) * 512], writes=[('zTs', n % 2)])
                            rhs_ap = zTs[:, n % 2, :]
                        else:
                            rhs_ap = src[0:kdim, n * 512:(n + 1) * 512]
                        S.op('pe', lambda: T.matmul(PS[bank][0:64, :], w_ap, rhs_ap, start=True, stop=True),
                             reads=['fw', ('zTs', n % 2), 'h1T'], writes=[psk(bank)])
                        t0, t1, t2 = ftmp[:, 0, :], ftmp[:, 1, :], ftmp[:, 2, :]
                        S.op('dve', lambda: V.tensor_scalar(out=t0, in0=PS[bank][0:64, :], scalar1=fcol[:, frc:frc + 1], scalar2=fcol[:, fbc:fbc + 1], op0=ALU.mult, op1=ALU.add),
                             reads=[psk(bank), 'fcol'], writes=['ft0'])
                        S.op('dve', lambda: V.tensor_scalar(out=t1, in0=t0, scalar1=PI, scalar2=-2 * PI, op0=ALU.is_gt, op1=ALU.mult), reads=['ft0'], writes=['ft1'])
                        S.op('dve', lambda: V.tensor_scalar(out=t2, in0=t0, scalar1=-PI, scalar2=2 * PI, op0=ALU.is_lt, op1=ALU.mult), reads=['ft0'], writes=['ft2'])
                        S.op('dve', lambda: V.tensor_tensor(out=t1, in0=t1, in1=t2, op=ALU.add), reads=['ft1', 'ft2'], writes=['ft1'])
                        S.op('dve', lambda: V.tensor_tensor(out=t0, in0=t0, in1=t1, op=ALU.add), reads=['ft0', 'ft1'], writes=['ft0'])
                        S.op('act', lambda: A.activation(out=dst[:, n * 512:(n + 1) * 512], in_=t0, func=AF.Sin), reads=['ft0'], writes=['h1T' if dst is h1T else 'h2T'])

                sin_layer(fw1[:, :], None, h1T, 1, 4, 33)
                sin_layer(fw2[:, :], h1T, h2T, 3, 5, 64)

                h2Tr = h1T
                S.op('dve', lambda: V.memset(h2Tr[:, 0:1], 0.0), reads=['h1T'], writes=['h1T'])
                S.op('dve', lambda: V.tensor_copy(out=h2Tr[:, 1:2048], in_=h2T[:, 2049:4096][:, ::-1]), reads=['h2T', 'h1T'], writes=['h1T'])
                for o in range(2):
                    for tc in range(16):
                        S.dma('sp', dct[:, 0, :, :], I['decay'][tc * 128:(tc + 1) * 128, :].rearrange("p (d o c) -> p d o c", d=2, o=2)[:, :, o, :], writes=[('dct', 0)])
                        S.dma('sp', dct[:, 1, :, :], I['decay_rev'][tc * 128:(tc + 1) * 128, :].rearrange("p (d o c) -> p d o c", d=2, o=2)[:, :, o, :], writes=[('dct', 1)])
                        for r, hsrc in enumerate((h2T, h2Tr)):
                            for d in range(2):
                                bank = r * 2 + d
                                S.op('pe', lambda: T.matmul(PS[bank][:, :], hsrc[:, tc * 128:(tc + 1) * 128], fw3[:, d * 1024 + o * 512: d * 1024 + o * 512 + 512], start=True, stop=True),
                                     reads=['h2T', 'h1T', 'fw'], writes=[psk(bank)])
                                S.op('dve', lambda: V.tensor_tensor(out=kft[:, r, d, :], in0=PS[bank][:, :], in1=dct[:, r, d, :], op=ALU.mult),
                                     reads=[psk(bank), ('dct', r)], writes=[('kft', r, d)])
                        if tc == 0:
                            S.op('dve', lambda: V.tensor_tensor(out=kft[0:1, 0, 0, :], in0=kft[0:1, 0, 0, :], in1=skr[0:1, o, :], op=ALU.add),
                                 reads=[('kft', 0, 0), 'skr'], writes=[('kft', 0, 0)])
                        kk = [('kft', r, d) for r in range(2) for d in range(2)]
                        for r in range(2):
                            S.op('dve', lambda: V.tensor_tensor(out=dct[:, r, 0, :], in0=kft[:, r, 0, :], in1=kft[:, r, 1, :], op=ALU.add), reads=kk + [('dct', r)], writes=[('dct', r)])
                            S.op('dve', lambda: V.tensor_tensor(out=dct[:, r, 1, :], in0=kft[:, r, 0, :], in1=kft[:, r, 1, :], op=ALU.subtract), reads=kk + [('dct', r)], writes=[('dct', r)])
                        dk = [('dct', 0), ('dct', 1)]
                        S.op('dve', lambda: V.tensor_tensor(out=eb[:, tc, :], in0=dct[:, 0, 0, :], in1=dct[:, 1, 0, :], op=ALU.add), reads=dk, writes=['eb'])
                        S.op('dve', lambda: V.tensor_tensor(out=eb[:, 16 + tc, :], in0=dct[:, 0, 0, :], in1=dct[:, 1, 0, :], op=ALU.subtract), reads=dk, writes=['eb'])
                        S.op('dve', lambda: V.tensor_tensor(out=ob[:, tc, :], in0=dct[:, 0, 1, :], in1=dct[:, 1, 1, :], op=ALU.add), reads=dk, writes=['ob'])
                        S.op('dve', lambda: V.tensor_tensor(out=ob[:, 16 + tc, :], in0=dct[:, 0, 1, :], in1=dct[:, 1, 1, :], op=ALU.subtract), reads=dk, writes=['ob'])
                    S.dma('sp', d2048[:, :, :], I['decay'][2048:2049, :].rearrange("p (d o c) -> p d o c", d=2, o=2)[:, :, o, :], writes=['d2048'])
                    for d in range(2):
                        S.op('pe', lambda: T.matmul(PS[d][0:1, :], h2T[:, 2048:2049], fw3[:, d * 1024 + o * 512: d * 1024 + o * 512 + 512], start=True, stop=True),
                             reads=['h2T', 'fw'], writes=[psk(d)])
                        S.op('dve', lambda: V.tensor_tensor(out=k2048[:, d, :], in0=PS[d][0:1, :], in1=d2048[:, d, :], op=ALU.mult), reads=[psk(d), 'd2048'], writes=['k2048'])
                    S.op('dve', lambda: V.tensor_tensor(out=eo2048[:, 0, :], in0=k2048[:, 0, :], in1=k2048[:, 1, :], op=ALU.add), reads=['k2048'], writes=['eo2048'])
                    S.op('dve', lambda: V.tensor_tensor(out=eo2048[:, 1, :], in0=k2048[:, 0, :], in1=k2048[:, 1, :], op=ALU.subtract), reads=['k2048'], writes=['eo2048'])
                    for fp in range(8):
                        even = fp < 4
                        S.dma('sp', rows[:, fp % 2, 0, :], I['dftc'][2048:2049, fp * 512:(fp + 1) * 512], writes=[('rows', fp % 2)])
                        S.dma('sp', rows[:, fp % 2, 1, :], I['dfts'][2048:2049, fp * 512:(fp + 1) * 512], writes=[('rows', fp % 2)])
                        for tc in range(16):
                            sl = (fp * 16 + tc) % 4
                            S.dma('sp', slab[:, sl, 0, :], I['dftc'][tc * 128:(tc + 1) * 128, fp * 512:(fp + 1) * 512], writes=[('slab', sl, 0)])
                            S.dma('sp', slab[:, sl, 1, :], I['dfts'][tc * 128:(tc + 1) * 128, fp * 512:(fp + 1) * 512], writes=[('slab', sl, 1)])
                            rc = eb[:, tc if even else 16 + tc, :]
                            rs = ob[:, 16 + tc if even else tc, :]
                            for fc in range(4):
                                S.op('pe', lambda: T.matmul(PS[fc][:, :], slab[:, sl, 0, fc * 128:(fc + 1) * 128], rc, start=(tc == 0), stop=(tc == 15 and not even)),
                                     reads=[('slab', sl, 0), 'eb'], writes=[psk(fc)], signal=(tc == 15))
                                last_s = (tc == 15) and even and not (fp == 0 and fc == 0)
                                S.op('pe', lambda: T.matmul(PS[4 + fc][:, :], slab[:, sl, 1, fc * 128:(fc + 1) * 128], rs, start=(tc == 0), stop=last_s),
                                     reads=[('slab', sl, 1), 'ob'], writes=[psk(4 + fc)], signal=(tc == 15 or fc == 3))
                        rk = [('rows', fp % 2), 'eo2048']
                        for fc in range(4):
                            if even:
                                S.op('pe', lambda: T.matmul(PS[fc][:, :], rows[0:1, fp % 2, 0, fc * 128:(fc + 1) * 128], eo2048[0:1, 0, :], start=False, stop=True), reads=rk, writes=[psk(fc)])
                            else:
                                S.op('pe', lambda: T.matmul(PS[4 + fc][:, :], rows[0:1, fp % 2, 1, fc * 128:(fc + 1) * 128], eo2048[0:1, 1, :], start=False, stop=True), reads=rk, writes=[psk(4 + fc)])
                        if fp == 0:
                            for tc in range(16):
                                S.op('pe', lambda: T.matmul(PS[4][:, :], nqs[:, tc, :], eb[:, tc, :], start=False, stop=False),
                                     reads=['nqs', 'eb'], writes=[psk(4)], signal=(tc == 15))
                            S.op('pe', lambda: T.matmul(PS[4][:, :], nqs[0:1, 0, :], eo2048[0:1, 0, :], start=False, stop=True), reads=['nqs', 'eo2048'], writes=[psk(4)])
                        for fc in range(4):
                            fcg = fp * 4 + fc
                            par = fcg % 2
                            S.op('act', lambda: A.activation(out=abo[:, par, 0, :], in_=PS[fc][:, :], func=AF.Copy, scale=wcol[:, fcg:fcg + 1]),
                                 reads=[psk(fc), 'wcol'], writes=[('abo', par, 0)])
                            S.op('act', lambda: A.activation(out=abo[:, par, 1, :], in_=PS[4 + fc][:, :], func=AF.Copy, scale=wcol[:, fcg:fcg + 1]),
                                 reads=[psk(4 + fc), 'wcol'], writes=[('abo', par, 1)])
                            S.dma('pool', AB_d[o, 0, fcg * 128:(fcg + 1) * 128, :], abo[:, par, 0, :], reads=[('abo', par, 0)], writes=[('AB', o)])
                            S.dma('pool', AB_d[o, 1, fcg * 128:(fcg + 1) * 128, :], abo[:, par, 1, :], reads=[('abo', par, 1)], writes=[('AB', o)])
                S.barrier()

        def phase_AB(l):
            with ExitStack() as es_:
                WA = es_.enter_context(nc.sbuf_tensor(UN("WA"), [128, 8, NWA * 128], BF16))
                for dc in range(8):
                    S.dma('pool', WA[:, dc, :], I['wa'][l, dc * 128:(dc + 1) * 128, :], writes=['WA'])
                load_gain(I['mix_norm'][l])
                for b in range(NBC):
                    xsrc = x_in[b] if l == 0 else xs[b]
                    with ExitStack() as es_:
                        ropec = es_.enter_context(nc.sbuf_tensor(UN("ropec"), [128, 2, 512], BF16))
                        ropes = es_.enter_context(nc.sbuf_tensor(UN("ropes"), [128, 2, 512], BF16))
                        qT = es_.enter_context(nc.sbuf_tensor(UN("qT"), [128, 4, L], BF16))
                        kTm = es_.enter_context(nc.sbuf_tensor(UN("kTm"), [128, 2, L], BF16))
                        vpad = es_.enter_context(nc.sbuf_tensor(UN("vpad"), [128, 2, 32, 128], BF16))
                        xt = es_.enter_context(nc.sbuf_tensor(UN("xt"), [128, 2, D], F32))
                        xn = es_.enter_context(nc.sbuf_tensor(UN("xn"), [128, 2, D], BF16))
                        hT2b = es_.enter_context(nc.sbuf_tensor(UN("hT"), [128, 2, 8, 512], BF16))
                        rawu = es_.enter_context(nc.sbuf_tensor(UN("rawu"), [128, 12, 514], BF16))
                        cw = es_.enter_context(nc.sbuf_tensor(UN("cw"), [128, 12, 3], F32))
                        cb = es_.enter_context(nc.sbuf_tensor(UN("cb"), [128, 12], F32))
                        ctmp = es_.enter_context(nc.sbuf_tensor(UN("ctmp"), [128, 2, 2, 512], F32))
                        uo = es_.enter_context(nc.sbuf_tensor(UN("uo"), [128, 2, 512], BF16))
                        sinkE = es_.enter_context(nc.sbuf_tensor(UN("sinkE"), [128, 4], F32))
                        PT = es_.enter_context(nc.sbuf_tensor(UN("PT"), [128, 2, 3, 512], BF16))
                        den = es_.enter_context(nc.sbuf_tensor(UN("den"), [128, 512], F32))
                        usb = es_.enter_context(nc.sbuf_tensor(UN("usb"), [128, 512], F32))
                        ao = es_.enter_context(nc.sbuf_tensor(UN("ao"), [128, 2, 512], BF16))
                        with nc.allow_non_contiguous_dma(reason="tiny"):
                            for k in range(3):
                                S.dma('sp', cw[:, :, k], I['cw'][l, k].rearrange("(j p) -> p j", p=128), writes=['cw'])
                            S.dma('sp', cb[:, :], I['cb'][l].rearrange("(j p) -> p j", p=128), writes=['cw'])
                            for kvh in range(2):
                                S.dma('sp', sinkE[kvh * 64:(kvh + 1) * 64, :], I['attn_sink'][l, kvh * 4:(kvh + 1) * 4].partition_broadcast(64), writes=['sinkE'])
                        S.op('act', lambda: A.activation(out=sinkE[:, :], in_=sinkE[:, :], func=AF.Exp), reads=['sinkE'], writes=['sinkE'])
                        S.op('dve', lambda: V.memset(kTm[:, :, :], 0.0), writes=['kTm'])
                        S.op('dve', lambda: V.memset(vpad[:, :, :, :], 0.0), writes=['vpad'])
                        S.op('dve', lambda: V.memset(rawu[:, :, :], 0.0), writes=[('rawu', j) for j in range(12)])

                        def conv_out(j, width, col0, par):
                            rk = ('rawu', j)
                            t1 = ctmp[:, par, 0, 0:width]
                            t2 = ctmp[:, par, 1, 0:width]
                            S.op('act', lambda: A.activation(out=t1, in_=rawu[:, j, 1:1 + width], func=AF.Identity, bias=cb[:, j:j + 1], scale=cw[:, j, 1:2]),
                                 reads=[rk, 'cw'], writes=[('ct', par, 0)])
                            S.op('dve', lambda: V.scalar_tensor_tensor(out=t2, in0=rawu[:, j, 0:width], scalar=cw[:, j, 0:1], in1=t1, op0=ALU.mult, op1=ALU.add),
                                 reads=[rk, 'cw', ('ct', par, 0)], writes=[('ct', par, 1)])
                            S.op('dve', lambda: V.scalar_tensor_tensor(out=uo[:, par, 0:width], in0=rawu[:, j, 2:2 + width], scalar=cw[:, j, 2:3], in1=t2, op0=ALU.mult, op1=ALU.add),
                                 reads=[rk, 'cw', ('ct', par, 1)], writes=[('uo', par)])
                            with nc.allow_non_contiguous_dma(reason="edge"):
                                S.dma('pool', U_d[b, j, :, col0:col0 + width], uo[:, par, 0:width], reads=[('uo', par)], writes=[('U', b, j)])

                        def front(n):
                            for t2 in range(2):
                                items = []
                                for tt in (2 * t2, 2 * t2 + 1):
                                    r0 = n * 512 + tt * 128
                                    S.dma('sp', xt[:, tt % 2, :], xsrc[r0:r0 + 128, :], writes=[('xt', tt % 2)])
                                    items.append((xt[:, tt % 2, :], ('xt', tt % 2), xn[:, tt % 2, :], ('xn', tt % 2), hT2b[:, n % 2, :, tt * 128:(tt + 1) * 128], ('hT', n % 2, tt), tt % 2))
                                norm_T_batch(items)

                        front(0)
                        for n in range(8):
                            tok0 = n * 512
                            hT = hT2b[:, n % 2]
                            S.dma('sp', ropec[:, n % 2, :], I['ropec'][:, tok0:tok0 + 512], writes=[('rope', n % 2)])
                            S.dma('sp', ropes[:, n % 2, :], I['ropes'][:, tok0:tok0 + 512], writes=[('rope', n % 2)])
                            hk = [('hT', n % 2, tt) for tt in range(4)]
                            for g in range(5):
                                ca, cs = (g, 4 + g) if g < 4 else (8, 9)
                                mm_group(2, [(WA[:, dc, ca * 128:(ca + 1) * 128], hT[:, dc, :]) for dc in range(8)], hk + ['WA'])
                                mm_group(3, [(WA[:, dc, cs * 128:(cs + 1) * 128], hT[:, dc, :]) for dc in range(8)], hk + ['WA'])
                                par = g % 2
                                t1 = ctmp[:, par, 0, :]
                                t2 = ctmp[:, par, 1, :]
                                S.op('dve', lambda: V.tensor_tensor(out=t1, in0=PS[2][:, :], in1=ropec[:, n % 2, :], op=ALU.mult), reads=[psk(2), ('rope', n % 2)], writes=[('ct', par, 0)])
                                S.op('dve', lambda: V.tensor_tensor(out=t2, in0=PS[3][:, :], in1=ropes[:, n % 2, :], op=ALU.mult), reads=[psk(3), ('rope', n % 2)], writes=[('ct', par, 1)])
                                if g < 4:
                                    S.op('dve', lambda: V.tensor_tensor(out=qT[:, g, tok0:tok0 + 512], in0=t1, in1=t2, op=ALU.add),
                                         reads=[('ct', par, 0), ('ct', par, 1)], writes=['qT'])
                                else:
                                    for kvh in range(2):
                                        sl = slice(kvh * 64, (kvh + 1) * 64)
                                        S.op('dve', lambda: V.tensor_tensor(out=kTm[sl, kvh, tok0:tok0 + 512], in0=ctmp[sl, par, 0, :], in1=ctmp[sl, par, 1, :], op=ALU.add),
                                             reads=[('ct', par, 0), ('ct', par, 1)], writes=['kTm'])
                            for tt in range(4):
                                for dc in range(8):
                                    S.op('pe', lambda: T.matmul(PS[4][:, tt * 128:(tt + 1) * 128], hT[:, dc, tt * 128:(tt + 1) * 128], WA[:, dc, 10 * 128:11 * 128], start=(dc == 0), stop=(dc == 7)),
                                         reads=hk + ['WA'], writes=[psk(4)], signal=(tt == 3 and dc == 7))
                            for kvh in range(2):
                                S.op('act', lambda: A.copy(out=vpad[:, kvh, n * 4:(n + 1) * 4, kvh * 64:(kvh + 1) * 64],
                                                           in_=PS[4][:, :].rearrange("p (t c) -> p t c", t=4)[:, :, kvh * 64:(kvh + 1) * 64]),
                                     reads=[psk(4)], writes=['vpad'])
                            if n + 1 < 8:
                                front(n + 1)
                            for j in range(12):
                                bank = (5, 6, 7, 2, 3, 4)[j % 6]
                                mm_group(bank, [(WA[:, dc, (11 + j) * 128:(12 + j) * 128], hT[:, dc, :]) for dc in range(8)], hk + ['WA'])
                                S.op('act', lambda: A.copy(out=rawu[:, j, 2:514], in_=PS[bank][:, :]), reads=[psk(bank)], writes=[('rawu', j)])
                                conv_out(j, 512, tok0, j % 2)
                                S.op('act', lambda: A.copy(out=rawu[:, j, 0:2], in_=rawu[:, j, 512:514]), reads=[('rawu', j)], writes=[('rawu', j)])
                        for j in range(12):
                            S.op('dve', lambda: V.memset(rawu[:, j, 2:3], 0.0), reads=[('rawu', j)], writes=[('rawu', j)])
                            conv_out(j, 1, L, j % 2)

                        units = [(i, kvh) for i in range(32) for kvh in range(2)]

                        def blocks_of(i):
                            return [j for j in (i - 1, i, i + 1) if 0 <= j < 32]

                        def scores(ui):
                            i, kvh = units[ui]
                            for jj, j in enumerate(blocks_of(i)):
                                bank = (ui % 2) * 3 + jj
                                S.op('pe', lambda: T.matmul(PS[bank][:, :], kTm[:, kvh, j * 128:(j + 1) * 128], qT[:, :, i * 128:(i + 1) * 128], start=True, stop=True),
                                     reads=['kTm', 'qT'], writes=[psk(bank)])

                        scores(0)
                        for ui, (i, kvh) in enumerate(units):
                            if ui + 1 < len(units):
                                scores(ui + 1)
                            blocks = blocks_of(i)
                            for jj, j in enumerate(blocks):
                                bank = (ui % 2) * 3 + jj
                                S.op('act', lambda: A.activation(out=PT[:, kvh, jj, :], in_=PS[bank][:, :], func=AF.Exp, scale=0.125),
                                     reads=[psk(bank)], writes=[('PT', kvh, jj)])
                                if j != i:
                                    mk = mprev if j < i else mnext
                                    pv = PT[:, kvh, jj, :].rearrange("p (g t) -> p g t", g=4)
                                    S.op('dve', lambda: V.tensor_tensor(out=pv, in0=pv, in1=mk[:, :].unsqueeze(1).broadcast_to([128, 4, 128]), op=ALU.mult),
                                         reads=[('PT', kvh, jj), 'mprev', 'mnext'], writes=[('PT', kvh, jj)])
                            nb_ = len(blocks)
                            for jj, j in enumerate(blocks):
                                first = (kvh == 0 and jj == 0)
                                lastm = (kvh == 1 and jj == nb_ - 1)
                                S.op('pe', lambda: T.matmul(PS[6][:, :], vpad[:, kvh, j, :], PT[:, kvh, jj, :], start=first, stop=lastm),
                                     reads=['vpad', ('PT', kvh, jj)], writes=[psk(6)], signal=lastm)
                                S.op('pe', lambda: T.matmul(PS[7][:, :], onesm[:, kvh, :], PT[:, kvh, jj, :], start=first, stop=lastm),
                                     reads=['onesm', ('PT', kvh, jj)], writes=[psk(7)], signal=(lastm or jj == nb_ - 1))
                            if kvh == 1:
                                S.op('act', lambda: A.copy(out=usb[:, :], in_=PS[6][:, :]), reads=[psk(6)], writes=['usb'])
                                S.op('act', lambda: A.copy(out=den[:, :], in_=PS[7][:, :]), reads=[psk(7)], writes=['den'])
                                S.op('dve', lambda: V.tensor_tensor(out=den[:, :].rearrange("p (g t) -> p g t", g=4), in0=den[:, :].rearrange("p (g t) -> p g t", g=4),
                                                                    in1=sinkE[:, :].unsqueeze(2).broadcast_to([128, 4, 128]), op=ALU.add),
                                     reads=['den', 'sinkE'], writes=['den'])
                                S.op('act', lambda: A.activation(out=den[:, :], in_=den[:, :], func=AF.Ln), reads=['den'], writes=['den'])
                                S.op('act', lambda: A.activation(out=den[:, :], in_=den[:, :], func=AF.Exp, scale=-1.0), reads=['den'], writes=['den'])
                                S.op('dve', lambda: V.tensor_tensor(out=ao[:, i % 2, :], in0=usb[:, :], in1=den[:, :], op=ALU.mult), reads=['usb', 'den'], writes=[('ao', i % 2)])
                                S.dma('pool', aT_d[b, :, :, i * 128:(i + 1) * 128], ao[:, i % 2, :].rearrange("p (g t) -> p g t", g=4), reads=[('ao', i % 2)], writes=[('aT', b)])
                        S.barrier()

        def phase_H(l):
            for b in range(NBC):
                with ExitStack() as es_:
                    vtm = es_.enter_context(nc.sbuf_tensor(UN("vtm"), [128, 32, 512], BF16))
                    Pr = es_.enter_context(nc.sbuf_tensor(UN("Pr"), [128, 32, 512], BF16))
                    Pq = es_.enter_context(nc.sbuf_tensor(UN("Pq"), [128, 32, 512], BF16))
                    hslab = es_.enter_context(nc.sbuf_tensor(UN("hslab"), [128, 6, 1024], BF16))
                    abt = es_.enter_context(nc.sbuf_tensor(UN("abt"), [128, 2, 4, 2, 512], BF16))
                    htmp = es_.enter_context(nc.sbuf_tensor(UN("htmp"), [128, 2, 2, 512], F32))
                    yo = es_.enter_context(nc.sbuf_tensor(UN("yo"), [128, 2, 512], BF16))
                    yo2 = es_.enter_context(nc.sbuf_tensor(UN("yo2"), [128, 2, 512], BF16))
                    zc = es_.enter_context(nc.sbuf_tensor(UN("zc"), [128, 4, 2, 512], F32))
                    gl2 = es_.enter_context(nc.sbuf_tensor(UN("gl2"), [128, 2, 4, 512], BF16))
                    ld2 = es_.enter_context(nc.sbuf_tensor(UN("ld2"), [128, 2, 512], BF16))
                    gl = es_.enter_context(nc.sbuf_tensor(UN("gl"), [128, 2, 4, 512], BF16))
                    ld = es_.enter_context(nc.sbuf_tensor(UN("ld"), [128, 2, 512], BF16))
                    def to_token_major(src_fn, srck):
                        for tg in range(4):
                            t0 = tg * 512
                            for cc in range(4):
                                par = (tg * 4 + cc) % 2
                                S.dma('sp', ld[:, par, :], src_fn(cc, t0), reads=[srck], writes=[('ld', par)])
                                S.dma('sp', ld2[:, par, :], src_fn(cc, 3584 - t0), reads=[srck], writes=[('ld2', par)])
                                S.op('dve', lambda: V.tensor_tensor(out=yo[:, par, :], in0=ld[:, par, :], in1=ld2[:, par, :][:, ::-1], op=ALU.add),
                                     reads=[('ld', par), ('ld2', par)], writes=[('yo', par)])
                                S.op('dve', lambda: V.tensor_tensor(out=yo2[:, par, :], in0=ld[:, par, :], in1=ld2[:, par, :][:, ::-1], op=ALU.subtract),
                                     reads=[('ld', par), ('ld2', par)], writes=[('yo2', par)])
                                for k in range(4):
                                    pbp = PS[k][:, :].bitcast(BF16)
                                    pbm = PS[4 + k][:, :].bitcast(BF16)
                                    S.op('pe', lambda: T.transpose(pbp[:, cc * 128:(cc + 1) * 128], yo[:, par, k * 128:(k + 1) * 128], identb[:, :]),
                                         reads=[('yo', par), 'identb'], writes=[psk(k)], signal=True)
                                    S.op('pe', lambda: T.transpose(pbm[:, cc * 128:(cc + 1) * 128], yo2[:, par, k * 128:(k + 1) * 128], identb[:, :]),
                                         reads=[('yo2', par), 'identb'], writes=[psk(4 + k)], signal=True)
                            for k in range(4):
                                S.op('act', lambda: A.copy(out=vtm[:, tg * 4 + k, :], in_=PS[k][:, :].bitcast(BF16)[:, 0:512]), reads=[psk(k)], writes=['vtm'])
                                S.op('dve', lambda: V.tensor_copy(out=vtm[:, 16 + tg * 4 + k, :], in_=PS[4 + k][:, :].bitcast(BF16)[:, 0:512]), reads=[psk(4 + k)], writes=['vtm'])

                    def forward(o):
                        for fp in range(8):
                            even = fp < 4
                            pp = fp % 2
                            for fc in range(4):
                                fcg = fp * 4 + fc
                                S.dma('pool', abt[:, pp, fc, 0, :], AB_d[o, 0, fcg * 128:(fcg + 1) * 128, :], reads=[('AB', o)], writes=[('abt', pp, fc, 0)])
                                S.dma('pool', abt[:, pp, fc, 1, :], AB_d[o, 1, fcg * 128:(fcg + 1) * 128, :], reads=[('AB', o)], writes=[('abt', pp, fc, 1)])
                            for tc in range(16):
                                sl = (fp * 16 + tc) % 6
                                S.dma('sp', hslab[:, sl, 0:512], I['dfcf'][tc * 128:(tc + 1) * 128, fp * 512:(fp + 1) * 512], writes=[('hs', sl, 0)])
                                S.dma('sp', hslab[:, sl, 512:1024], I['dfsf'][tc * 128:(tc + 1) * 128, fp * 512:(fp + 1) * 512], writes=[('hs', sl, 1)])
                                rc = vtm[:, tc if even else 16 + tc, :]
                                rs = vtm[:, 16 + tc if even else tc, :]
                                for fc in range(4):
                                    S.op('pe', lambda: T.matmul(PS[fc][:, :], hslab[:, sl, fc * 128:(fc + 1) * 128], rc, start=(tc == 0), stop=(tc == 15)),
                                         reads=[('hs', sl, 0), 'vtm'], writes=[psk(fc)], signal=(tc == 15))
                                    S.op('pe', lambda: T.matmul(PS[4 + fc][:, :], hslab[:, sl, 512 + fc * 128:512 + (fc + 1) * 128], rs, start=(tc == 0), stop=(tc == 15)),
                                         reads=[('hs', sl, 1), 'vtm'], writes=[psk(4 + fc)], signal=(tc == 15 or fc == 3))
                            for fc in range(4):
                                fcg = fp * 4 + fc
                                par = fcg % 2
                                Zr, Zs = zc[:, fc, 0, :], zc[:, fc, 1, :]
                                S.op('act', lambda: A.copy(out=Zr, in_=PS[fc][:, :]), reads=[psk(fc)], writes=[('zc', fc, 0)])
                                S.op('act', lambda: A.copy(out=Zs, in_=PS[4 + fc][:, :]), reads=[psk(4 + fc)], writes=[('zc', fc, 1)])
                                Am, Bm = abt[:, pp, fc, 0, :], abt[:, pp, fc, 1, :]
                                t1, t2 = htmp[:, par, 0, :], htmp[:, par, 1, :]
                                kz = [('zc', fc, 0), ('zc', fc, 1), ('abt', pp, fc, 0), ('abt', pp, fc, 1)]
                                S.op('dve', lambda: V.tensor_tensor(out=t1, in0=Zr, in1=Am, op=ALU.mult), reads=kz, writes=[('ht', par, 0)])
                                S.op('dve', lambda: V.tensor_tensor(out=t2, in0=Zs, in1=Bm, op=ALU.mult), reads=kz, writes=[('ht', par, 1)])
                                S.op('dve', lambda: V.tensor_tensor(out=Pr[:, fcg, :], in0=t1, in1=t2, op=ALU.subtract), reads=[('ht', par, 0), ('ht', par, 1)], writes=['Pr'])
                                S.op('dve', lambda: V.tensor_tensor(out=t1, in0=Zr, in1=Bm, op=ALU.mult), reads=kz, writes=[('ht', par, 0)])
                                S.op('dve', lambda: V.tensor_tensor(out=t2, in0=Zs, in1=Am, op=ALU.mult), reads=kz, writes=[('ht', par, 1)])
                                S.op('dve', lambda: V.tensor_tensor(out=Pq[:, fcg, :], in0=t1, in1=t2, op=ALU.add), reads=[('ht', par, 0), ('ht', par, 1)], writes=['Pq'])
                                if fcg == 0:
                                    S.op('dve', lambda: V.tensor_tensor(out=Pr[0:1, 0, :], in0=Zr[0:1, :], in1=Am[0:1, :], op=ALU.mult), reads=kz + ['Pr'], writes=['Pr'])
                                    S.op('dve', lambda: V.tensor_tensor(out=Pq[0:1, 0, :], in0=Zs[0:1, :], in1=Bm[0:1, :], op=ALU.mult), reads=kz + ['Pq'], writes=['Pq'])

                    def inverse(gate_j, dst_fn, dstk):
                        for tp in range(4):
                            t0 = tp * 512
                            pp = tp % 2
                            for cc in range(4):
                                gk = ('U', b, gate_j * 4 + cc)
                                S.dma('pool', gl[:, pp, cc, :], U_d[b, gate_j * 4 + cc, :, 1 + t0:1 + t0 + 512], reads=[gk], writes=[('gl', pp, cc)])
                                S.dma('pool', gl2[:, pp, cc, :], U_d[b, gate_j * 4 + cc, :, 1 + (3584 - t0):1 + (3584 - t0) + 512], reads=[gk], writes=[('gl2', pp, cc)])
                            for k in range(32):
                                sl = (tp * 32 + k) % 6
                                S.dma('sp', hslab[:, sl, 0:512], I['dfci'][k * 128:(k + 1) * 128, t0:t0 + 512], writes=[('hs', sl, 0)])
                                S.dma('sp', hslab[:, sl, 512:1024], I['dfsi'][k * 128:(k + 1) * 128, t0:t0 + 512], writes=[('hs', sl, 1)])
                                even = k < 16
                                Cs, Ss = hslab[:, sl, 0:512], hslab[:, sl, 512:1024]
                                for cc in range(4):
                                    Al, Ar = (Pr, Cs) if even else (Pq, Ss)
                                    Bl, Br = (Pq, Ss) if even else (Pr, Cs)
                                    rk = [('hs', sl, 0), ('hs', sl, 1), 'Pr', 'Pq']
                                    S.op('pe', lambda: T.matmul(PS[cc * 2][:, :], Al[:, k, cc * 128:(cc + 1) * 128], Ar, start=(k == 0), stop=(k == 31)),
                                         reads=rk, writes=[psk(cc * 2)], signal=(k == 31))
                                    S.op('pe', lambda: T.matmul(PS[cc * 2 + 1][:, :], Bl[:, k, cc * 128:(cc + 1) * 128], Br, start=(k == 0), stop=(k == 31)),
                                         reads=rk, writes=[psk(cc * 2 + 1)], signal=(k == 31 or cc == 3))
                            for cc in range(4):
                                par = cc % 2
                                u0 = 3584 - t0
                                gk = ('U', b, gate_j * 4 + cc)
                                Bs, Sm = htmp[:, par, 0, :], htmp[:, par, 1, :]
                                Ac, Bc = zc[:, cc, 0, :], zc[:, cc, 1, :]
                                S.op('act', lambda: A.copy(out=Ac, in_=PS[cc * 2][:, :]), reads=[psk(cc * 2)], writes=[('zc', cc, 0)])
                                S.op('act', lambda: A.copy(out=Bc, in_=PS[cc * 2 + 1][:, :]), reads=[psk(cc * 2 + 1)], writes=[('zc', cc, 1)])
                                S.op('dve', lambda: V.tensor_tensor(out=Sm, in0=Ac, in1=Bc, op=ALU.add), reads=[('zc', cc, 0), ('zc', cc, 1)], writes=[('ht', par, 1)])
                                S.op('dve', lambda: V.tensor_tensor(out=Bs, in0=Ac, in1=Bc, op=ALU.subtract), reads=[('zc', cc, 0), ('zc', cc, 1)], writes=[('ht', par, 0)])
                                S.op('dve', lambda: V.tensor_tensor(out=yo[:, par, :], in0=Sm, in1=gl[:, pp, cc, :], op=ALU.mult), reads=[('ht', par, 1), ('gl', pp, cc)], writes=[('yo', par)])
                                S.op('dve', lambda: V.tensor_tensor(out=yo2[:, par, :], in0=Bs[:, ::-1], in1=gl2[:, pp, cc, :], op=ALU.mult), reads=[('ht', par, 0), ('gl2', pp, cc)], writes=[('yo2', par)])
                                S.dma('pool', dst_fn(cc, t0), yo[:, par, :], reads=[('yo', par)], writes=[dstk])
                                S.dma('pool', dst_fn(cc, u0), yo2[:, par, :], reads=[('yo2', par)], writes=[dstk])

                    to_token_major(lambda cc, t0: U_d[b, 8 + cc, :, 1 + t0:1 + t0 + 512], ('U', b, 8))
                    forward(0)
                    inverse(0, lambda cc, t0: ZZ_d[cc, :, t0:t0 + 512], 'ZZ')
                    to_token_major(lambda cc, t0: ZZ_d[cc, :, t0:t0 + 512], 'ZZ')
                    forward(1)
                    inverse(1, lambda cc, t0: yT_d[b, :, cc, t0:t0 + 512], ('yT', b))
                    S.barrier()

        def phase_O(l):
            with ExitStack() as es_:
                WO = es_.enter_context(nc.sbuf_tensor(UN("WO"), [128, 8, D], BF16))
                wst = es_.enter_context(nc.sbuf_tensor(UN("wst"), [128, 2, D], F32))
                gcolo = es_.enter_context(nc.sbuf_tensor(UN("gcolo"), [128, 8], F32))
                xo = es_.enter_context(nc.sbuf_tensor(UN("xo"), [128, 2, D], F32))
                at = es_.enter_context(nc.sbuf_tensor(UN("at"), [128, 2, 2, 4, 128], BF16))
                sq = es_.enter_context(nc.sbuf_tensor(UN("sq"), [128, 2, 2, 4, 128], BF16))
                rs = es_.enter_context(nc.sbuf_tensor(UN("rs"), [128, 2, 2], F32))
                with nc.allow_non_contiguous_dma(reason="tiny"):
                    S.dma('sp', gcolo[:, 0:4], I['ga'][l].rearrange("(k p) -> p k", p=128), writes=['gcolo'])
                    S.dma('sp', gcolo[:, 4:8], I['gy'][l].rearrange("(k p) -> p k", p=128), writes=['gcolo'])
                for k in range(8):
                    S.dma('sp', wst[:, k % 2, :], I['w_out'][l, k * 128:(k + 1) * 128, :], writes=[('wst', k % 2)])
                    S.op('dve', lambda: V.tensor_scalar(out=WO[:, k, :], in0=wst[:, k % 2, :], scalar1=gcolo[:, k:k + 1], scalar2=None, op0=ALU.mult),
                         reads=[('wst', k % 2), 'gcolo'], writes=['WO'])
                for b in range(NBC):
                    xsrc = x_in[b] if l == 0 else xs[b]
                    for i in range(32):
                        par = i % 2
                        r0 = i * 128
                        S.dma('sp', xo[:, par, :], xsrc[r0:r0 + 128, :], writes=[('xo', par)])
                        S.dma('sp', at[:, par, 0, :, :], aT_d[b, :, :, r0:r0 + 128], writes=[('at', par, 0)])
                        S.dma('sp', at[:, par, 1, :, :], yT_d[b, :, :, r0:r0 + 128], writes=[('at', par, 1)])
                        for w in range(2):
                            S.op('act', lambda: A.activation(out=sq[:, par, w, :, :], in_=at[:, par, w, :, :], func=AF.Square), reads=[('at', par, w)], writes=[('sq', par, w)])
                            for g in range(4):
                                S.op('pe', lambda: T.matmul(PS[4 + w][:, 0:1], sq[:, par, w, g, :], onesb[:, 0:1], start=(g == 0), stop=(g == 3)),
                                     reads=[('sq', par, w), 'onesb'], writes=[psk(4 + w)], signal=(g == 3))
                            S.op('act', lambda: A.activation(out=rs[:, par, w:w + 1], in_=PS[4 + w][:, 0:1], func=AF.Ln, bias=epsc[:, 0:1], scale=1.0 / 512),
                                 reads=[psk(4 + w)], writes=[('rs', par, w)])
                            S.op('act', lambda: A.activation(out=rs[:, par, w:w + 1], in_=rs[:, par, w:w + 1], func=AF.Exp, scale=-0.5), reads=[('rs', par, w)], writes=[('rs', par, w)])
                            for half in range(2):
                                bank = w * 2 + half
                                mm_group(bank, [(at[:, par, w, g, :], WO[:, w * 4 + g, half * 512:(half + 1) * 512]) for g in range(4)], [('at', par, w), 'WO'])
                                S.op('dve', lambda: V.scalar_tensor_tensor(out=xo[:, par, half * 512:(half + 1) * 512], in0=PS[bank][:, :], scalar=rs[:, par, w:w + 1],
                                                                           in1=xo[:, par, half * 512:(half + 1) * 512], op0=ALU.mult, op1=ALU.add),
                                     reads=[psk(bank), ('rs', par, w), ('xo', par)], writes=[('xo', par)])
                        S.dma('pool', xs[b, r0:r0 + 128, :], xo[:, par, :], reads=[('xo', par)], writes=[('xs', b)])
                S.barrier()

        def phase_XF(l):
            last = (l == n_layers - 1)
            moe = (l % 2 == 1)
            nfc = (DFE if moe else DFF) // 128
            with ExitStack() as es_:
                XQ = es_.enter_context(nc.sbuf_tensor(UN("XQ"), [128, 8, 512], BF16))
                XO = es_.enter_context(nc.sbuf_tensor(UN("XO"), [128, 4, D], BF16))
                gx = es_.enter_context(nc.sbuf_tensor(UN("gx"), [128, D], F32))
                gf = es_.enter_context(nc.sbuf_tensor(UN("gf"), [128, D], F32))
                xt2 = es_.enter_context(nc.sbuf_tensor(UN("xt2"), [128, 4, D], F32))
                xn2 = es_.enter_context(nc.sbuf_tensor(UN("xn2"), [128, 4, D], BF16))
                hT2 = es_.enter_context(nc.sbuf_tensor(UN("hT2"), [128, 8, 512], BF16))
                qx = es_.enter_context(nc.sbuf_tensor(UN("qx"), [128, 4, 512], BF16))
                PTx = es_.enter_context(nc.sbuf_tensor(UN("PTx"), [128, 2, 2, 512], BF16))
                ox = es_.enter_context(nc.sbuf_tensor(UN("ox"), [128, 4, 512], BF16))
                rdn = es_.enter_context(nc.sbuf_tensor(UN("rdn"), [128, 2, 512], F32))
                wgs = es_.enter_context(nc.sbuf_tensor(UN("wgs"), [128, 3, 8, 128], BF16))
                wus = es_.enter_context(nc.sbuf_tensor(UN("wus"), [128, 3, 8, 128], BF16))
                wds = es_.enter_context(nc.sbuf_tensor(UN("wds"), [128, 3, D], BF16))
                actT = es_.enter_context(nc.sbuf_tensor(UN("actT"), [128, 28, 512], BF16))
                sg = es_.enter_context(nc.sbuf_tensor(UN("sg"), [128, 2, 512], F32))
                hn32 = es_.enter_context(nc.sbuf_tensor(UN("hn32"), [128, D], F32))
                hT32 = es_.enter_context(nc.sbuf_tensor(UN("hT32"), [128, 8, 128], F32))
                identf = es_.enter_context(nc.sbuf_tensor(UN("identf"), [128, 128], F32))
                wr = es_.enter_context(nc.sbuf_tensor(UN("wr"), [128, 8, NE], F32))
                lg = es_.enter_context(nc.sbuf_tensor(UN("lg"), [128, 4, NE], F32))
                m8 = es_.enter_context(nc.sbuf_tensor(UN("m8"), [128, 4, 8], F32))
                gw = es_.enter_context(nc.sbuf_tensor(UN("gw"), [128, 4, NE], F32))
                gtmp = es_.enter_context(nc.sbuf_tensor(UN("gtmp"), [128, 4, NE], F32))
                yfin = es_.enter_context(nc.sbuf_tensor(UN("yfin"), [128, 2, D], F32))
                with ExitStack() as es_in:
                    XK = es_in.enter_context(nc.sbuf_tensor(UN("XK"), [128, 8, 512], BF16))
                    XV = es_in.enter_context(nc.sbuf_tensor(UN("XV"), [128, 8, 512], BF16))
                    for nm, tl in (('xw_q', XQ), ('xw_k', XK), ('xw_v', XV)):
                        S.dma('pool', tl[:, :, :], I[nm][l].rearrange("(k p) n -> p k n", p=128), writes=[nm])
                    S.dma('pool', XO[:, :, :], I['xw_o'][l].rearrange("(k p) n -> p k n", p=128), writes=['xw_o'])
                    S.op('act', lambda: A.copy(out=identf[:, :], in_=identb[:, :]), reads=['identb'], writes=['identf'])
                    with nc.allow_non_contiguous_dma(reason="small"):
                        S.dma('sp', wr[:, :, :], I['moe_router'].rearrange("(k p) e -> p k e", p=128), writes=['wr'])
                        S.dma('sp', gx[:, :], I['xattn_norm'][l].partition_broadcast(128), writes=['gx'])
                        S.dma('sp', gf[:, :], I['ffn_norm'][l].partition_broadcast(128), writes=['gf'])
                        if last:
                            S.dma('sp', gbc[:, :], I['final_norm'].partition_broadcast(128), writes=['gbc'])
                    for h in range(4):
                        mm_group(h % 2, [(XK[:, dc, h * 128:(h + 1) * 128], mT[:, dc, :]) for dc in range(8)], ['xw_k'])
                        S.op('act', lambda: A.copy(out=KmT[:, h, :], in_=PS[h % 2][:, :]), reads=[psk(h % 2)], writes=['KmT'])
                    for bj in range(NBC * 2):
                        mm_group(2 + bj % 2, [(mT[:, dc, bj * 128:(bj + 1) * 128], XV[:, dc, :]) for dc in range(8)], ['xw_v'])
                        S.op('act', lambda: A.copy(out=Vm[:, bj, :], in_=PS[2 + bj % 2][:, :]), reads=[psk(2 + bj % 2)], writes=['Vm'])
                    S.barrier()

                wctr = [0]
                Mcat = es_.enter_context(nc.sbuf_tensor(UN("Mcat"), [128, 64, 16], F32))
                Wts = es_.enter_context(nc.sbuf_tensor(UN("Wts"), [128, 64, 2], F32))
                sloti = es_.enter_context(nc.sbuf_tensor(UN("sloti"), [128, 128], mybir.dt.int32))
                eblki = es_.enter_context(nc.sbuf_tensor(UN("eblki"), [128, NBLK], mybir.dt.int32))

                def moe_sparse(last):
                    with ExitStack() as es2:
                        sb = lambda nm, shp, dt: es2.enter_context(nc.sbuf_tensor(UN(nm), shp, dt))
                        ltb = sb("ltb", [128, 128], BF16); slt = sb("slt", [64, 65], BF16)
                        thr32 = sb("thr32", [128, 32], F32); thr40 = sb("thr40", [128, NBLK], F32)
                        Mb = sb("Mb", [128, 64, 16], BF16); Mke = sb("Mke", [128, 16, 64], BF16)
                        within = sb("within", [128, 64, 16], F32)
                        totTs = sb("totTs", [64, 16], F32); totB = sb("totB", [64, 16, 128], BF16)
                        offs = sb("offs", [128, 16, 65], F32)
                        nn = sb("nn", [128, 8], F32); nb = sb("nb", [128, 8], F32); base = sb("base", [128, 9], F32)
                        cK = sb("cK", [128, 16], F32); cmp = sb("cmp", [128, NBLK], F32); eb = sb("ebf", [128, NBLK], F32)
                        sv = sb("sv", [128, 64, 16], F32); slotf = sb("slotf", [128, 128], F32)
                        zb = actT[:, 0:4, :].rearrange("p (n a) b -> p n (a b)", n=2)
                        yg = sb("yg", [128, 2, 2, D], F32)
                        rowb = sb("rowb", [128, DFE // 128], F32); widxf = sb("widxf", [128, 2, DFE // 128], F32); widxi = sb("widxi", [128, 2, DFE // 128], mybir.dt.int32)
                        S.dma('sp', rowb[:, :], I['rowbase'], writes=['rowb'])
                        S.dma('sp', ltb[:, :], I['ltb'], writes=['ltb'])
                        S.dma('sp', slt[:, :], I['slt65'], writes=['slt'])
                        S.dma('sp', thr32[:, :], I['thr32'], writes=['thr'])
                        S.dma('sp', thr40[:, :], I['thr40'], writes=['thr'])
                        S.op('dve', lambda: V.memset(zb[:, :, :], 0.0), writes=['zb'])
                        Gv = G_d.rearrange("(n p) d -> p n d", p=128)
                        for k in range(NBLK * 4 // 2):
                            S.dma('sp', Gv[:, k * 2:(k + 1) * 2, :], zb[:, :, :], reads=['zb'], writes=[('Gz', k)])
                        S.op('dve', lambda: V.tensor_copy(out=Mb[:, :, :], in_=Mcat[:, :, :]), reads=['Mcat'], writes=['Mb'])
                        S.op('dve', lambda: V.tensor_copy(out=Mke[:, :, :], in_=Mcat[:, :, :].rearrange("p t k -> p k t")), reads=['Mcat'], writes=['Mke'])
                        Mb2 = Mb[:, :, :].rearrange("p t k -> p (t k)")
                        for hh in range(2):
                            S.op('pe', lambda: T.matmul(PS[hh][:, :], ltb[:, :], Mb2[:, hh * 512:(hh + 1) * 512], start=True, stop=True), reads=['ltb', 'Mb'], writes=[psk(hh)])
                            S.op('dve', lambda: V.tensor_copy(out=within[:, :, :].rearrange("p t k -> p (t k)")[:, hh * 512:(hh + 1) * 512], in_=PS[hh][:, :]), reads=[psk(hh)], writes=['within'])
                        for ke in range(16):
                            S.op('pe', lambda: T.matmul(PS[2][0:64, ke:ke + 1], Mke[:, ke, :], onesb[:, 0:1], start=True, stop=True), reads=['Mke', 'onesb'], writes=[psk(2)], signal=(ke == 15))
                        S.op('dve', lambda: V.tensor_copy(out=totTs[:, :], in_=PS[2][0:64, 0:16]), reads=[psk(2)], writes=['totTs'])
                        S.op('dve', lambda: V.tensor_copy(out=totB[:, :, :], in_=totTs[:, :].unsqueeze(2).broadcast_to([64, 16, 128])), reads=['totTs'], writes=['totB'])
                        for ke in range(16):
                            bank, col = 3 + ke // 6, (ke % 6) * 65
                            S.op('pe', lambda: T.matmul(PS[bank][:, col:col + 65], totB[:, ke, :], slt[:, :], start=True, stop=True), reads=['totB', 'slt'], writes=[psk(bank)], signal=True)
                        for g3 in range(3):
                            nk = 6 if g3 < 2 else 4
                            S.op('dve', lambda: V.tensor_copy(out=offs[:, g3 * 6:g3 * 6 + nk, :], in_=PS[3 + g3][:, 0:nk * 65].rearrange("p (k t) -> p k t", k=nk)), reads=[psk(3 + g3)], writes=['offs'])
                        S.op('dve', lambda: V.tensor_tensor(out=nn[:, :], in0=offs[:, 0:8, 64], in1=offs[:, 8:16, 64], op=ALU.add), reads=['offs'], writes=['nn'])
                        for e in range(NE):
                            S.op('dve', lambda: V.tensor_scalar(out=cmp[:, 0:32], in0=thr32[:, :], scalar1=nn[:, e:e + 1], scalar2=None, op0=ALU.is_lt), reads=['thr', 'nn', 'cmp'], writes=['cmp'])
                            S.op('dve', lambda: V.reduce_sum(out=nb[:, e:e + 1], in_=cmp[:, 0:32], axis=mybir.AxisListType.X), reads=['cmp'], writes=['nb'])
                        S.op('dve', lambda: V.memset(base[:, 0:1], 0.0), writes=['base'])
                        for e in range(NE):
                            S.op('dve', lambda: V.scalar_tensor_tensor(out=base[:, e + 1:e + 2], in0=nb[:, e:e + 1], scalar=512.0, in1=base[:, e:e + 1], op0=ALU.mult, op1=ALU.add), reads=['nb', 'base'], writes=['base'])
                        S.op('dve', lambda: V.tensor_copy(out=cK[:, 0:8], in_=base[:, 0:8]), reads=['base'], writes=['cK'])
                        S.op('dve', lambda: V.tensor_tensor(out=cK[:, 8:16], in0=base[:, 0:8], in1=offs[:, 0:8, 64], op=ALU.add), reads=['base', 'offs'], writes=['cK'])
                        S.op('dve', lambda: V.tensor_tensor(out=sv[:, :, :], in0=within[:, :, :], in1=offs[:, :, 0:64].rearrange("p k t -> p t k"), op=ALU.add), reads=['within', 'offs'], writes=['sv'])
                        S.op('dve', lambda: V.tensor_tensor(out=sv[:, :, :], in0=sv[:, :, :], in1=cK[:, :].unsqueeze(1).broadcast_to([128, 64, 16]), op=ALU.add), reads=['sv', 'cK'], writes=['sv'])
                        S.op('dve', lambda: V.tensor_tensor(out=sv[:, :, :], in0=sv[:, :, :], in1=Mcat[:, :, :], op=ALU.mult), reads=['sv', 'Mcat'], writes=['sv'])
                        S.op('dve', lambda: V.reduce_sum(out=slotf[:, :], in_=sv[:, :, :].rearrange("p t (k e) -> p (t k) e", k=2), axis=mybir.AxisListType.X), reads=['sv'], writes=['slotf'])
                        S.op('dve', lambda: V.tensor_copy(out=sloti[:, :], in_=slotf[:, :]), reads=['slotf'], writes=['sloti'])
                        S.op('dve', lambda: V.memset(eb[:, :], 0.0), writes=['ebf'])
                        for e in range(NE):
                            S.op('dve', lambda: V.tensor_scalar(out=cmp[:, :], in0=thr40[:, :], scalar1=base[:, e + 1:e + 2], scalar2=None, op0=ALU.is_gt), reads=['thr', 'base', 'cmp'], writes=['cmp'])
                            S.op('dve', lambda: V.tensor_tensor(out=eb[:, :], in0=eb[:, :], in1=cmp[:, :], op=ALU.add), reads=['cmp', 'ebf'], writes=['ebf'])
                        S.op('dve', lambda: V.tensor_scalar(out=eb[:, :], in0=eb[:, :], scalar1=float(NE - 1), scalar2=None, op0=ALU.min), reads=['ebf'], writes=['ebf'])
                        S.op('dve', lambda: V.tensor_scalar(out=eb[:, :], in0=eb[:, :], scalar1=float(DFE), scalar2=None, op0=ALU.mult), reads=['ebf'], writes=['ebf'])
                        S.barrier()
                        for tg in range(64):
                            par = tg % 2
                            S.dma('sp', xn2[:, par, :], H2_d[tg * 128:(tg + 1) * 128, :], writes=[('xn2', par)])
                            for k in range(2):
                                S.idma(G_d[:, :], bass.IndirectOffsetOnAxis(ap=sloti[:, tg * 2 + k:tg * 2 + k + 1], axis=0), xn2[:, par, :], None,
                                       reads=[('xn2', par), 'sloti'], writes=['G'])
                        S.barrier()
                        def load_block(j):
                            for tt in range(4):
                                S.dma('sp', xn2[:, tt, :], G_d[j * 512 + tt * 128: j * 512 + (tt + 1) * 128, :], reads=['G'], writes=[('xn2', tt)])
                                pb = PS[tt % 2][:, :].bitcast(BF16)
                                for k in range(8):
                                    S.op('pe', lambda: T.transpose(pb[:, k * 128:(k + 1) * 128], xn2[:, tt, k * 128:(k + 1) * 128], identb[:, :]),
                                         reads=[('xn2', tt), 'identb'], writes=[psk(tt % 2)], signal=(k == 7))
                                S.op('act', lambda: A.copy(out=hT2[:, :, tt * 128:(tt + 1) * 128], in_=pb.rearrange("p (k t) -> p k t", k=8)), reads=[psk(tt % 2)], writes=[('hT2', tt)])

                        load_block(0)
                        for j in range(NBLK):
                            wp = j % 2
                            S.op('dve', lambda: V.tensor_scalar(out=widxf[:, wp, :], in0=rowb[:, :], scalar1=eb[:, j:j + 1], scalar2=None, op0=ALU.add), reads=['ebf', 'rowb', ('widxf', wp)], writes=[('widxf', wp)])
                            S.op('dve', lambda: V.tensor_copy(out=widxi[:, wp, :], in_=widxf[:, wp, :]), reads=[('widxf', wp)], writes=[('widxi', wp)])
                            ffn_expert(None, None, None, lambda tt: None, sink=(widxi[:, wp, :], ('widxi', wp)))
                            if j + 1 < NBLK:
                                load_block(j + 1)
                            S.dma('sp', Y_d[j * 512:(j + 1) * 512, :].rearrange("(t p) d -> p t d", p=128), xt2[:, :, :], reads=[('xt2', tt) for tt in range(4)], writes=['Y'])
                        S.barrier()
                        for tg in range(64):
                            par = tg % 2
                            b, r0 = tg // 32, (tg % 32) * 128
                            xk = ('xt2', par)
                            S.dma('sp', xt2[:, par, :], xs[b, r0:r0 + 128, :], writes=[xk])
                            for k in range(2):
                                S.idma(yg[:, par, k, :], None, Y_d[:, :], bass.IndirectOffsetOnAxis(ap=sloti[:, tg * 2 + k:tg * 2 + k + 1], axis=0),
                                       reads=['sloti', 'Y'], writes=[('yg', par, k)])
                                S.op('dve', lambda: V.scalar_tensor_tensor(out=xt2[:, par, :], in0=yg[:, par, k, :], scalar=Wts[:, tg, k:k + 1], in1=xt2[:, par, :], op0=ALU.mult, op1=ALU.add),
                                     reads=[('yg', par, k), 'Wts', xk], writes=[xk])
                            if not last:
                                S.dma('sp', xs[b, r0:r0 + 128, :], xt2[:, par, :], reads=[xk], writes=[('xs', b)])
                            else:
                                si = ss_ctr[0] % 8
                                ss_ctr[0] += 1
                                sc = ss[:, si:si + 1]
                                sk = ('ss', si)
                                S.op('act', lambda: A.activation(out=junk[:, :], in_=xt2[:, par, :], func=AF.Square, accum_out=sc), reads=[xk], writes=['junk', sk])
                                S.op('act', lambda: A.activation(out=sc, in_=sc, func=AF.Ln, bias=epsc[:, 0:1], scale=1.0 / D), reads=[sk], writes=[sk])
                                S.op('act', lambda: A.activation(out=sc, in_=sc, func=AF.Exp, scale=-0.5), reads=[sk], writes=[sk])
                                S.op('dve', lambda: V.scalar_tensor_tensor(out=yfin[:, par, :], in0=xt2[:, par, :], scalar=sc, in1=gbc[:, :], op0=ALU.mult, op1=ALU.mult),
                                     reads=[xk, sk, 'gbc'], writes=[('yfin', par)])
                                S.dma('sp', y_out[b, r0:r0 + 128, :], yfin[:, par, :], reads=[('yfin', par)], writes=['y'])

                def ffn_expert(wgv, wuv, wd_fn, gcol_fn, sink=None, mid_hook=None):
                    hk = [('hT2', tt) for tt in range(4)]
                    dense_w = sink is None
                    for fcn in range(nfc):
                        sl = wctr[0] % 3
                        wctr[0] += 1
                        if dense_w:
                            S.dma('pool', wgs[:, sl, :, :].rearrange("p k f -> p (k f)"), wgv(fcn), writes=[('wgs', sl)])
                            S.dma('pool', wus[:, sl, :, :].rearrange("p k f -> p (k f)"), wuv(fcn), writes=[('wus', sl)])
                        else:
                            ioff = bass.IndirectOffsetOnAxis(ap=sink[0][:, fcn:fcn + 1], axis=0)
                            S.idma(wgs[:, sl, :, :].rearrange("p k f -> p (k f)"), None, I['moe_wg'], ioff, reads=[sink[1]], writes=[('wgs', sl)])
                            S.idma(wus[:, sl, :, :].rearrange("p k f -> p (k f)"), None, I['moe_wu'], ioff, reads=[sink[1]], writes=[('wus', sl)])
                        bg, bu = 2 * (fcn % 2), 2 * (fcn % 2) + 1
                        mm_group(bg, [(wgs[:, sl, dc, :], hT2[:, dc, :]) for dc in range(8)], hk + [('wgs', sl)])
                        mm_group(bu, [(wus[:, sl, dc, :], hT2[:, dc, :]) for dc in range(8)], hk + [('wus', sl)])
                        S.op('act', lambda: A.activation(out=sg[:, fcn % 2, :], in_=PS[bg][:, :], func=AF.Silu), reads=[psk(bg)], writes=[('sg', fcn % 2)])
                        S.op('dve', lambda: V.tensor_tensor(out=actT[:, fcn, :], in0=PS[bu][:, :], in1=sg[:, fcn % 2, :], op=ALU.mult),
                             reads=[psk(bu), ('sg', fcn % 2)], writes=[('actT', fcn)])
                    if mid_hook is not None:
                        mid_hook()
                    for fcn in range(nfc):
                        sl = wctr[0] % 3
                        wctr[0] += 1
                        if dense_w:
                            S.dma('pool', wds[:, sl, :], wd_fn(fcn), writes=[('wds', sl)])
                        else:
                            S.idma(wds[:, sl, :], None, I['moe_wd'], bass.IndirectOffsetOnAxis(ap=sink[0][:, fcn:fcn + 1], axis=0), reads=[sink[1]], writes=[('wds', sl)])
                        for tt in range(4):
                            for half in range(2):
                                S.op('pe', lambda: T.matmul(PS[tt * 2 + half][:, :], actT[:, fcn, tt * 128:(tt + 1) * 128], wds[:, sl, half * 512:(half + 1) * 512],
                                                            start=(fcn == 0), stop=(fcn == nfc - 1)),
                                     reads=[('actT', fcn), ('wds', sl)], writes=[psk(tt * 2 + half)], signal=(fcn == nfc - 1 or (tt == 3 and half == 1)))
                    for tt in range(4):
                        for half in range(2):
                            xv = xt2[:, tt, half * 512:(half + 1) * 512]
                            gc = gcol_fn(tt)
                            if sink is not None:
                                S.op('act' if half else 'dve', lambda: (A.copy if half else V.tensor_copy)(out=xv, in_=PS[tt * 2 + half][:, :]), reads=[psk(tt * 2 + half)], writes=[('xt2', tt)])
                            elif gc is None:
                                S.op('dve', lambda: V.tensor_tensor(out=xv, in0=PS[tt * 2 + half][:, :], in1=xv, op=ALU.add), reads=[psk(tt * 2 + half), ('xt2', tt)], writes=[('xt2', tt)])
                            else:
                                S.op('dve', lambda: V.scalar_tensor_tensor(out=xv, in0=PS[tt * 2 + half][:, :], scalar=gc, in1=xv, op0=ALU.mult, op1=ALU.add),
                                     reads=[psk(tt * 2 + half), ('xt2', tt), 'gw'], writes=[('xt2', tt)])

                for b in range(NBC):
                    for n in range(8):
                        tok0 = n * 512
                        S.dma('sp', xt2[:, :, :], xs[b, tok0:tok0 + 512, :].rearrange("(t p) d -> p t d", p=128), reads=[('xs', b)], writes=[('xt2', tt) for tt in range(4)])
                        norm_T_batch([(xt2[:, tt, :], ('xt2', tt), xn2[:, tt, :], ('xn2', tt), hT2[:, :, tt * 128:(tt + 1) * 128], ('hT2', tt), tt % 2) for tt in range(4)], gx, 'gx')
                        hk = [('hT2', tt) for tt in range(4)]
                        for h in range(4):
                            mm_group(2 + h % 2, [(XQ[:, dc, h * 128:(h + 1) * 128], hT2[:, dc, :]) for dc in range(8)], hk + ['xw_q'])
                            S.op('act', lambda: A.copy(out=qx[:, h, :], in_=PS[2 + h % 2][:, :]), reads=[psk(2 + h % 2)], writes=[('qx', h)])
                        def xscores(h):
                            for j in range(2):
                                bank = (4, 2)[h % 2] + j
                                S.op('pe', lambda: T.matmul(PS[bank][:, :], KmT[:, h, b * 256 + j * 128: b * 256 + (j + 1) * 128], qx[:, h, :], start=True, stop=True),
                                     reads=['KmT', ('qx', h)], writes=[psk(bank)])

                        xscores(0)
                        for h in range(4):
                            hp = h % 2
                            if h + 1 < 4:
                                xscores(h + 1)
                            for j in range(2):
                                bank = (4, 2)[hp] + j
                                S.op('act', lambda: A.activation(out=PTx[:, hp, j, :], in_=PS[bank][:, :], func=AF.Exp, scale=128 ** -0.5), reads=[psk(bank)], writes=[('PTx', hp, j)])
                            rk = [('PTx', hp, 0), ('PTx', hp, 1), 'Vm', 'onesb']
                            mm_group(6, [(Vm[:, b * 2 + j, h * 128:(h + 1) * 128], PTx[:, hp, j, :]) for j in range(2)], rk)
                            mm_group(7, [(onesb[:, :], PTx[:, hp, j, :]) for j in range(2)], rk)
                            S.op('act', lambda: A.activation(out=rdn[:, hp, :], in_=PS[7][:, :], func=AF.Ln), reads=[psk(7)], writes=[('rdn', hp)])
                            S.op('act', lambda: A.activation(out=rdn[:, hp, :], in_=rdn[:, hp, :], func=AF.Exp, scale=-1.0), reads=[('rdn', hp)], writes=[('rdn', hp)])
                            S.op('dve', lambda: V.tensor_tensor(out=ox[:, h, :], in0=PS[6][:, :], in1=rdn[:, hp, :], op=ALU.mult), reads=[psk(6), ('rdn', hp)], writes=[('ox', h)])
                        for tt in range(4):
                            for half in range(2):
                                mm_group(half, [(ox[:, h, tt * 128:(tt + 1) * 128], XO[:, h, half * 512:(half + 1) * 512]) for h in range(4)], [('ox', h) for h in range(4)] + ['xw_o'])
                                xv = xt2[:, tt, half * 512:(half + 1) * 512]
                                S.op('dve', lambda: V.tensor_tensor(out=xv, in0=PS[half][:, :], in1=xv, op=ALU.add), reads=[psk(half), ('xt2', tt)], writes=[('xt2', tt)])
                        if not moe:
                            norm_T_batch([(xt2[:, tt, :], ('xt2', tt), xn2[:, tt, :], ('xn2', tt), hT2[:, :, tt * 128:(tt + 1) * 128], ('hT2', tt), tt % 2) for tt in range(4)], gf, 'gf')
                            ffn_expert(lambda fcn: I['ffn_wg'][fcn * 128:(fcn + 1) * 128, :], lambda fcn: I['ffn_wu'][fcn * 128:(fcn + 1) * 128, :], lambda fcn: I['ffn_wd'][fcn * 128:(fcn + 1) * 128, :], lambda tt: None)
                        else:
                            for tt in range(4):
                                si = ss_ctr[0] % 8
                                ss_ctr[0] += 1
                                sc = ss[:, si:si + 1]
                                sk = ('ss', si)
                                xk = ('xt2', tt)
                                S.op('act', lambda: A.activation(out=junk[:, :], in_=xt2[:, tt, :], func=AF.Square, accum_out=sc), reads=[xk], writes=['junk', sk])
                                S.op('act', lambda: A.activation(out=sc, in_=sc, func=AF.Ln, bias=epsc[:, 0:1], scale=1.0 / D), reads=[sk], writes=[sk])
                                S.op('act', lambda: A.activation(out=sc, in_=sc, func=AF.Exp, scale=-0.5), reads=[sk], writes=[sk])
                                S.op('dve', lambda: V.scalar_tensor_tensor(out=hn32[:, :], in0=xt2[:, tt, :], scalar=sc, in1=gf[:, :], op0=ALU.mult, op1=ALU.mult),
                                     reads=[xk, sk, 'gf'], writes=['hn32'])
                                for k in range(8):
                                    S.op('pe', lambda: T.transpose(PS[k // 4][:, (k % 4) * 128:(k % 4 + 1) * 128], hn32[:, k * 128:(k + 1) * 128], identf[:, :]),
                                         reads=['hn32', 'identf'], writes=[psk(k // 4)], signal=(k % 4 == 3))
                                for hb in range(2):
                                    S.op('act', lambda: A.copy(out=hT32[:, hb * 4:(hb + 1) * 4, :], in_=PS[hb][:, :].rearrange("p (k t) -> p k t", k=4)), reads=[psk(hb)], writes=['hT32'])
                                for dc in range(8):
                                    S.op('pe', lambda: T.matmul(PS[2][:, 0:NE], hT32[:, dc, :], wr[:, dc, :], start=(dc == 0), stop=(dc == 7)),
                                         reads=['hT32', 'wr'], writes=[psk(2)], signal=(dc == 7))
                                S.op('dve', lambda: V.tensor_copy(out=lg[:, tt, :], in_=PS[2][:, 0:NE]), reads=[psk(2)], writes=['lg'])
                                S.op('dve', lambda: V.max(out=m8[:, tt, :], in_=lg[:, tt, :]), reads=['lg'], writes=['m8'])
                                tile_g = (b * 8 + n) * 4 + tt
                                S.op('dve', lambda: V.tensor_scalar(out=Mcat[:, tile_g, 0:8], in0=lg[:, tt, :], scalar1=m8[:, tt, 0:1], scalar2=None, op0=ALU.is_equal), reads=['lg', 'm8'], writes=['Mcat'])
                                S.op('dve', lambda: V.tensor_scalar(out=Mcat[:, tile_g, 8:16], in0=lg[:, tt, :], scalar1=m8[:, tt, 1:2], scalar2=None, op0=ALU.is_equal), reads=['lg', 'm8'], writes=['Mcat'])
                                S.op('dve', lambda: V.tensor_tensor(out=m8[:, tt, 2:3], in0=m8[:, tt, 1:2], in1=m8[:, tt, 0:1], op=ALU.subtract), reads=['m8'], writes=['m8'])
                                S.op('act', lambda: A.activation(out=m8[:, tt, 2:3], in_=m8[:, tt, 2:3], func=AF.Exp), reads=['m8'], writes=['m8'])
                                S.op('dve', lambda: V.tensor_scalar(out=m8[:, tt, 2:3], in0=m8[:, tt, 2:3], scalar1=1.0, scalar2=None, op0=ALU.add), reads=['m8'], writes=['m8'])
                                S.op('dve', lambda: V.reciprocal(out=Wts[:, tile_g, 0:1], in_=m8[:, tt, 2:3]), reads=['m8'], writes=['Wts'])
                                S.op('dve', lambda: V.tensor_scalar(out=Wts[:, tile_g, 1:2], in0=Wts[:, tile_g, 0:1], scalar1=-1.0, scalar2=1.0, op0=ALU.mult, op1=ALU.add), reads=['Wts'], writes=['Wts'])
                                S.op('act', lambda: A.copy(out=xn2[:, tt % 2, :], in_=hn32[:, :]), reads=['hn32'], writes=[('xn2', tt % 2)])
                                r0 = b * L + tok0 + tt * 128
                                S.dma('sp', H2_d[r0:r0 + 128, :], xn2[:, tt % 2, :], reads=[('xn2', tt % 2)], writes=['H2'])
                        if moe or not last:
                            S.dma('sp', xs[b, tok0:tok0 + 512, :].rearrange("(t p) d -> p t d", p=128), xt2[:, :, :], reads=[('xt2', tt) for tt in range(4)], writes=[('xs', b)])
                        if last and not moe:
                            for tt in range(4):
                                si = ss_ctr[0] % 8
                                ss_ctr[0] += 1
                                sc = ss[:, si:si + 1]
                                sk = ('ss', si)
                                xk = ('xt2', tt)
                                S.op('act', lambda: A.activation(out=junk[:, :], in_=xt2[:, tt, :], func=AF.Square, accum_out=sc), reads=[xk], writes=['junk', sk])
                                S.op('act', lambda: A.activation(out=sc, in_=sc, func=AF.Ln, bias=epsc[:, 0:1], scale=1.0 / D), reads=[sk], writes=[sk])
                                S.op('act', lambda: A.activation(out=sc, in_=sc, func=AF.Exp, scale=-0.5), reads=[sk], writes=[sk])
                                S.op('dve', lambda: V.scalar_tensor_tensor(out=yfin[:, tt % 2, :], in0=xt2[:, tt, :], scalar=sc, in1=gbc[:, :], op0=ALU.mult, op1=ALU.mult),
                                     reads=[xk, sk, 'gbc'], writes=[('yfin', tt % 2)])
                                S.dma('sp', y_out[b, tok0 + tt * 128: tok0 + (tt + 1) * 128, :], yfin[:, tt % 2, :], reads=[('yfin', tt % 2)], writes=['y'])
                if moe:
                    moe_sparse(last)
                S.barrier()
        with ExitStack() as es_:
            junk = es_.enter_context(nc.sbuf_tensor(UN("junk"), [128, D], BF16))
            S.dma('sp', identb[:, :], I['identb'], writes=['identb'])
            S.dma('sp', mprev[:, :], I['mprev'], writes=['mprev'])
            S.dma('sp', mnext[:, :], I['mnext'], writes=['mnext'])
            S.op('dve', lambda: V.memset(onesb[:, :], 1.0), writes=['onesb'])
            S.op('dve', lambda: V.memset(onesm[:, :, :], 0.0), writes=['onesm'])
            S.op('dve', lambda: V.memset(onesm[:, 0, 0:64], 1.0), reads=['onesm'], writes=['onesm'])
            S.op('dve', lambda: V.memset(onesm[:, 1, 64:128], 1.0), reads=['onesm'], writes=['onesm'])
            S.op('dve', lambda: V.memset(epsc[:, :], EPS), writes=['epsc'])
            S.op('dve', lambda: V.memset(npi[:, :], -PI), writes=['npi'])
            S.barrier()

            phase_mem()
            for l in range(n_layers):
                phase_F(l)
                phase_AB(l)
                phase_H(l)
                phase_O(l)
                phase_XF(l)
            S.barrier()
    return nc


_PROG = {}


def _slab_layout(w):
    E, Dd, F = w.shape
    return np.ascontiguousarray(w.reshape(E, Dd // 128, 128, F // 128, 128).transpose(0, 3, 2, 1, 4)).reshape(E * F, Dd)


def _prep_inputs(inp):
    c = host_constants()
    f32 = lambda a: np.ascontiguousarray(np.asarray(a, dtype=np.float32))
    perm = attn_row_perm()
    w_out = f32(inp['w_out']).copy()
    w_out[:, :512, :] = w_out[:, perm, :]
    shared = {
        'mem_norm': f32(inp['mem_norm']), 'mix_norm': f32(inp['mix_norm']),
        'wa': np.stack([permute_w_in(f32(inp['w_in'])[l]) for l in range(2)]),
        'attn_sink': f32(inp['attn_sink']), 'cw': f32(inp['hy_conv_w']), 'cb': f32(inp['hy_conv_b']),
        'f_w1': f32(inp['hy_f_w1']), 'f_b1': f32(inp['hy_f_b1']), 'f_fr1': f32(inp['hy_f_freq1']),
        'f_w2': f32(inp['hy_f_w2']), 'f_b2': f32(inp['hy_f_b2']), 'f_fr2': f32(inp['hy_f_freq2']),
        'f_w3': f32(inp['hy_f_w3']), 'skip': f32(inp['hy_skip']),
        'ga': np.ascontiguousarray(f32(inp['attn_out_norm'])[:, perm]), 'gy': f32(inp['hy_out_norm']),
        'w_out': w_out, 'xattn_norm': f32(inp['xattn_norm']),
        'xw_q': f32(inp['xw_q']), 'xw_k': f32(inp['xw_k']), 'xw_v': f32(inp['xw_v']), 'xw_o': f32(inp['xw_o']),
        'ffn_norm': f32(inp['ffn_norm']), 'ffn_wg': _slab_layout(f32(inp['ffn_w_gate'])), 'ffn_wu': _slab_layout(f32(inp['ffn_w_up'])),
        'ffn_wd': f32(inp['ffn_w_down'])[0], 'moe_router': f32(inp['moe_router'])[0],
        'moe_wg': _slab_layout(f32(inp['moe_w_gate'])[0]), 'moe_wu': _slab_layout(f32(inp['moe_w_up'])[0]), 'moe_wd': f32(inp['moe_w_down'])[0].reshape(NE * DFE, D),
        'final_norm': f32(inp['final_norm']),
    }
    for k in ('identb', 'mprev', 'mnext', 'ropec', 'ropes', 'dftc', 'dfts', 'dftnq', 'dfcf', 'dfsf', 'dfci', 'dfsi', 'wcol', 'zT', 'decay', 'decay_rev', 'ltb', 'slt65', 'thr32', 'thr40', 'rowbase'):
        shared[k] = c[k]
    return shared


def kernel(**inputs):
    x = np.asarray(inputs['x'], dtype=np.float32)
    mem = np.asarray(inputs['mem'], dtype=np.float32)
    shared = _prep_inputs(inputs)
    if 'nc' not in _PROG:
        _PROG['nc'] = build_program()
    nc = _PROG['nc']
    in_maps = []
    for c in range(NCORES):
        m = dict(shared)
        m['x'] = np.ascontiguousarray(x[c * NBC:(c + 1) * NBC])
        m['mem'] = np.ascontiguousarray(mem[c * NBC:(c + 1) * NBC].reshape(NBC * MEM, D))
        in_maps.append(m)
    res = run_bass_kernel_spmd(nc, in_maps, core_ids=list(range(NCORES)))
    return np.concatenate([np.asarray(r['y']) for r in res.results], axis=0).astype(np.float32)
```
